# Optimizing a Trainium2 kernel written in Bass

```python
import jax, jax.numpy as jnp
from jax import lax
import numpy as np

D_MODEL = 2048
BATCH = 4
SEQ = 2048
DEPTH = 2

GRID_W = 64
CTX_LEN = 256
N_BRANCH = 4
BRANCH_W = D_MODEL // N_BRANCH
HEAD_DIM = 64
N_HEADS = BRANCH_W // HEAD_DIM
N_KV_HEADS = 2
WINDOW = 128
BLOCK = 128
ROPE_THETA = 10000.0
SGU_CHUNK = 128
SGU_GROUPS = 4
SGU_GROUP_W = BRANCH_W // SGU_GROUPS
SCONV_K = 3
CONF_K = 31
N_EXPERTS = 16
N_GROUPS = 4
EXPERTS_PER_GROUP = N_EXPERTS // N_GROUPS
TOP_K = 2
D_FF_EXPERT = D_MODEL // 2
Q_W = N_HEADS * HEAD_DIM
KV_W = N_KV_HEADS * HEAD_DIM
SECTION_WIDTHS = (Q_W, KV_W, KV_W, BRANCH_W, BRANCH_W, BRANCH_W, BRANCH_W, BRANCH_W, BRANCH_W, BRANCH_W, N_BRANCH * D_MODEL)
N_IN = Q_W + 2 * KV_W + 7 * BRANCH_W + N_BRANCH * D_MODEL

kernel_name = "hybrid_parallel_gated_mixers_grouped_moe_dit"

F32 = jnp.float32


def rmsnorm(x, g, eps=1e-6):
    xf = x.astype(F32)
    y = xf * lax.rsqrt(jnp.mean(xf * xf, axis=-1, keepdims=True) + eps)
    return (y * g.astype(F32)).astype(x.dtype)


def layernorm(x, g, b, eps=1e-5):
    xf = x.astype(F32)
    mu = jnp.mean(xf, axis=-1, keepdims=True)
    var = jnp.mean(jnp.square(xf - mu), axis=-1, keepdims=True)
    y = (xf - mu) * lax.rsqrt(var + eps) * g.astype(F32) + b.astype(F32)
    return y.astype(x.dtype)


def split_sections(p):
    pts, acc = [], 0
    for w in SECTION_WIDTHS[:-1]:
        acc += w
        pts.append(acc)
    return jnp.split(p, pts, axis=-1)


def axial_rope_tables(n):
    rows = n // GRID_W
    row = jnp.broadcast_to(jnp.arange(rows)[:, None], (rows, GRID_W)).reshape(-1)
    col = jnp.broadcast_to(jnp.arange(GRID_W)[None, :], (rows, GRID_W)).reshape(-1)
    half = HEAD_DIM // 2
    inv = 1.0 / (ROPE_THETA ** (jnp.arange(0, half, 2, dtype=F32) / half))
    ang_r = row.astype(F32)[:, None, None] * inv
    ang_c = col.astype(F32)[:, None, None] * inv
    return jnp.cos(ang_r), jnp.sin(ang_r), jnp.cos(ang_c), jnp.sin(ang_c)


def rotate(x, cos, sin):
    xf = x.astype(F32)
    x1, x2 = jnp.split(xf, 2, axis=-1)
    return jnp.concatenate([x1 * cos - x2 * sin, x2 * cos + x1 * sin], axis=-1).astype(x.dtype)


def axial_rope(x, tabs):
    cr, sr, cc, sc = tabs
    half = HEAD_DIM // 2
    return jnp.concatenate([rotate(x[..., :half], cr, sr), rotate(x[..., half:], cc, sc)], axis=-1)


def window_attention(q, k, v, kc, vc, sink):
    b, s, h, dh = q.shape
    g = k.shape[2]
    r = h // g
    nb = s // BLOCK
    scale = dh ** -0.5
    qb = q.reshape(b, nb, BLOCK, g, r, dh)

    def band(t):
        tp = jnp.pad(t, ((0, 0), (BLOCK, BLOCK), (0, 0), (0, 0))).reshape(b, nb + 2, BLOCK, g, dh)
        return jnp.concatenate([tp[:, :-2], tp[:, 1:-1], tp[:, 2:]], axis=2)

    kb, vb = band(k), band(v)
    s_loc = jnp.einsum('bnqgrd,bnkgd->bngrqk', qb, kb).astype(F32) * scale
    qpos = jnp.arange(nb)[:, None] * BLOCK + jnp.arange(BLOCK)[None, :]
    kpos = jnp.arange(nb)[:, None] * BLOCK - BLOCK + jnp.arange(3 * BLOCK)[None, :]
    valid = (jnp.abs(qpos[:, :, None] - kpos[:, None, :]) <= WINDOW) & (kpos[:, None, :] >= 0) & (kpos[:, None, :] < s)
    s_loc = jnp.where(valid[None, :, None, None], s_loc, -jnp.inf)
    s_ctx = jnp.einsum('bnqgrd,bmgd->bngrqm', qb, kc).astype(F32) * scale
    sink_b = jnp.broadcast_to(sink.astype(F32).reshape(g, r)[None, None, :, :, None, None], s_loc.shape[:-1] + (1,))
    p = jax.nn.softmax(jnp.concatenate([s_loc, s_ctx, sink_b], axis=-1), axis=-1)
    nk = 3 * BLOCK
    lc = kc.shape[1]
    p_loc = p[..., :nk].astype(v.dtype)
    p_ctx = p[..., nk:nk + lc].astype(v.dtype)
    o = jnp.einsum('bngrqk,bnkgd->bnqgrd', p_loc, vb) + jnp.einsum('bngrqm,bmgd->bnqgrd', p_ctx, vc)
    return o.reshape(b, s, h * dh)


def context_attention(qc, kc, vc, sink):
    b, l, h, dh = qc.shape
    g = kc.shape[2]
    r = h // g
    qg = qc.reshape(b, l, g, r, dh)
    s = jnp.einsum('blgrd,bmgd->bgrlm', qg, kc).astype(F32) * (dh ** -0.5)
    sink_b = jnp.broadcast_to(sink.astype(F32).reshape(g, r)[None, :, :, None, None], s.shape[:-1] + (1,))
    p = jax.nn.softmax(jnp.concatenate([s, sink_b], axis=-1), axis=-1)
    o = jnp.einsum('bgrlm,bmgd->blgrd', p[..., :l].astype(vc.dtype), vc)
    return o.reshape(b, l, h * dh)


def chunk_sgu(u, v, ln_g, ln_b, w_s, b_s):
    b, t, w = u.shape
    u = jax.nn.gelu(u)
    v = layernorm(jax.nn.gelu(v), ln_g, ln_b)
    vr = v.reshape(b, t // SGU_CHUNK, SGU_CHUNK, SGU_GROUPS, SGU_GROUP_W)
    sp = jnp.einsum('gpq,bnqgc->bnpgc', w_s, vr) + b_s.T[:, :, None]
    return u * sp.reshape(b, t, w)


def dwconv(x, w):
    kw = w.shape[0]
    return lax.conv_general_dilated(x, w[:, None, :].astype(x.dtype), window_strides=(1,),
                                    padding=((kw // 2, kw // 2),),
                                    dimension_numbers=('NWC', 'WIO', 'NWC'),
                                    feature_group_count=x.shape[-1])


def conformer_conv(a, bgate, dw_w, dw_b, ln_g, ln_b):
    z = a * jax.nn.sigmoid(bgate)
    z = dwconv(z, dw_w) + dw_b
    z = layernorm(z, ln_g, ln_b)
    return jax.nn.silu(z)


def branch_mix(attn_y, su, sv, cb, cc, cx, ga, gb, gates,
               sgu_ln_g, sgu_ln_b, sgu_w, sgu_b, sconv_w,
               conf_dw_w, conf_dw_b, conf_ln_g, conf_ln_b, w_branch, w_out):
    y_b = chunk_sgu(su, sv, sgu_ln_g, sgu_ln_b, sgu_w, sgu_b)
    y_c = cb * dwconv(cc * cx, sconv_w)
    y_d = conformer_conv(ga, gb, conf_dw_w, conf_dw_b, conf_ln_g, conf_ln_b)
    gates = jax.nn.sigmoid(gates.astype(F32)).astype(attn_y.dtype)
    merged = 0.0
    for i, y in enumerate((attn_y, y_b, y_c, y_d)):
        merged = merged + gates[..., i * D_MODEL:(i + 1) * D_MODEL] * (y @ w_branch[i])
    return merged @ w_out


def moe(h, router_w, router_b, w_up, w_down):
    t = h.shape[0]
    scores = jax.nn.softmax((h @ router_w).astype(F32), axis=-1)
    biased = scores + router_b.astype(F32)
    grp_score = lax.top_k(biased.reshape(t, N_GROUPS, EXPERTS_PER_GROUP), TOP_K)[0].sum(-1)
    gsel = jnp.argmax(grp_score, axis=-1)
    in_group = (jnp.arange(N_EXPERTS) // EXPERTS_PER_GROUP)[None, :] == gsel[:, None]
    _, idx = lax.top_k(jnp.where(in_group, biased, -jnp.inf), TOP_K)
    wsel = jnp.take_along_axis(scores, idx, axis=-1)
    wsel = wsel / jnp.sum(wsel, axis=-1, keepdims=True)
    comb = jnp.sum(jax.nn.one_hot(idx, N_EXPERTS, dtype=F32) * wsel[..., None], axis=1).astype(h.dtype)
    y = jnp.zeros_like(h)
    for e in range(N_EXPERTS):
        a, b = jnp.split(h @ w_up[e], 2, axis=-1)
        y = y + comb[:, e:e + 1] * ((jax.nn.silu(a) * b) @ w_down[e])
    return y


def setup_inputs(seed: int = 0) -> dict:
    key = jax.random.key(seed)
    ks = jax.random.split(key, 32)

    def nrm(k, shape, scale):
        return jax.random.normal(k, shape, jnp.float32) * scale

    D = D_MODEL
    return {
        "x": nrm(ks[0], (BATCH, SEQ, D), 1.0),
        "c": nrm(ks[1], (BATCH, D), 1.0),
        "ctx": nrm(ks[2], (BATCH, CTX_LEN, D), 1.0),
        "c_ctx": nrm(ks[3], (D,), 1.0),
        "ada_w": nrm(ks[4], (DEPTH, D, 6 * D), 0.3 * D ** -0.5),
        "ada_b": nrm(ks[5], (DEPTH, 6 * D), 0.01),
        "norm1_g": 1.0 + nrm(ks[6], (DEPTH, D), 0.02),
        "norm2_g": 1.0 + nrm(ks[7], (DEPTH, D), 0.02),
        "w_in": nrm(ks[8], (DEPTH, D, N_IN), D ** -0.5),
        "attn_sink": nrm(ks[9], (DEPTH, N_HEADS), 0.5),
        "sgu_ln_g": 1.0 + nrm(ks[10], (DEPTH, BRANCH_W), 0.02),
        "sgu_ln_b": nrm(ks[11], (DEPTH, BRANCH_W), 0.02),
        "sgu_w": nrm(ks[12], (DEPTH, SGU_GROUPS, SGU_CHUNK, SGU_CHUNK), SGU_CHUNK ** -0.5),
        "sgu_b": 1.0 + nrm(ks[13], (DEPTH, SGU_GROUPS, SGU_CHUNK), 0.02),
        "sconv_w": nrm(ks[14], (DEPTH, SCONV_K, BRANCH_W), SCONV_K ** -0.5),
        "conf_dw_w": nrm(ks[15], (DEPTH, CONF_K, BRANCH_W), CONF_K ** -0.5),
        "conf_dw_b": nrm(ks[16], (DEPTH, BRANCH_W), 0.02),
        "conf_ln_g": 1.0 + nrm(ks[17], (DEPTH, BRANCH_W), 0.02),
        "conf_ln_b": nrm(ks[18], (DEPTH, BRANCH_W), 0.02),
        "w_branch": nrm(ks[19], (DEPTH, N_BRANCH, BRANCH_W, D), BRANCH_W ** -0.5),
        "w_out": nrm(ks[20], (DEPTH, D, D), D ** -0.5),
        "router_w": nrm(ks[21], (D, N_EXPERTS), D ** -0.5),
        "router_b": nrm(ks[22], (N_EXPERTS,), 0.01),
        "exp_w_up": nrm(ks[23], (DEPTH, N_EXPERTS, D, 2 * D_FF_EXPERT), D ** -0.5),
        "exp_w_down": nrm(ks[24], (DEPTH, N_EXPERTS, D_FF_EXPERT, D), D_FF_EXPERT ** -0.5),
        "final_g": 1.0 + nrm(ks[25], (D,), 0.02),
    }


def reference(x, c, ctx, c_ctx, ada_w, ada_b, norm1_g, norm2_g, w_in, attn_sink,
              sgu_ln_g, sgu_ln_b, sgu_w, sgu_b, sconv_w, conf_dw_w, conf_dw_b,
              conf_ln_g, conf_ln_b, w_branch, w_out, router_w, router_b,
              exp_w_up, exp_w_down, final_g):
    b, s, d = x.shape
    l_ctx = ctx.shape[1]
    tabs = axial_rope_tables(s)
    cx = ctx
    silu_c = jax.nn.silu(c)
    silu_cc = jax.nn.silu(c_ctx)[None, :]
    for l in range(DEPTH):
        last = l == DEPTH - 1
        sh1, sc1, g1, sh2, sc2, g2 = jnp.split((silu_c @ ada_w[l] + ada_b[l])[:, None, :], 6, axis=-1)
        sh1c, sc1c, g1c, sh2c, sc2c, g2c = jnp.split((silu_cc @ ada_w[l] + ada_b[l])[:, None, :], 6, axis=-1)
        lp = (sgu_ln_g[l], sgu_ln_b[l], sgu_w[l], sgu_b[l], sconv_w[l],
              conf_dw_w[l], conf_dw_b[l], conf_ln_g[l], conf_ln_b[l], w_branch[l], w_out[l])

        hx = rmsnorm(x, norm1_g[l]) * (1 + sc1) + sh1
        hc = rmsnorm(cx, norm1_g[l]) * (1 + sc1c) + sh1c
        px = split_sections(hx @ w_in[l])
        pc = split_sections(hc @ w_in[l])
        kc = pc[1].reshape(b, l_ctx, N_KV_HEADS, HEAD_DIM)
        vc = pc[2].reshape(b, l_ctx, N_KV_HEADS, HEAD_DIM)
        q = axial_rope(px[0].reshape(b, s, N_HEADS, HEAD_DIM), tabs)
        k = axial_rope(px[1].reshape(b, s, N_KV_HEADS, HEAD_DIM), tabs)
        v = px[2].reshape(b, s, N_KV_HEADS, HEAD_DIM)
        ya = window_attention(q, k, v, kc, vc, attn_sink[l])
        x_new = x + g1 * branch_mix(ya, *px[3:], *lp)
        if not last:
            yac = context_attention(pc[0].reshape(b, l_ctx, N_HEADS, HEAD_DIM), kc, vc, attn_sink[l])
            cx = cx + g1c * branch_mix(yac, *pc[3:], *lp)
        x = x_new

        hx2 = (rmsnorm(x, norm2_g[l]) * (1 + sc2) + sh2).reshape(b * s, d)
        if not last:
            hc2 = (rmsnorm(cx, norm2_g[l]) * (1 + sc2c) + sh2c).reshape(b * l_ctx, d)
            tokens = jnp.concatenate([hx2, hc2], axis=0)
        else:
            tokens = hx2
        yt = moe(tokens, router_w, router_b, exp_w_up[l], exp_w_down[l])
        x = x + g2 * yt[:b * s].reshape(b, s, d)
        if not last:
            cx = cx + g2c * yt[b * s:].reshape(b, l_ctx, d)
    return rmsnorm(x, final_g)
```

```python
import math
from contextlib import ExitStack
import numpy as np
import concourse.bass as bass
import concourse.mybir as mybir
from concourse.bass_utils import run_bass_kernel_spmd

F32 = mybir.dt.float32
BF16 = mybir.dt.bfloat16
U8 = mybir.dt.uint8
AF = mybir.ActivationFunctionType
ALU = mybir.AluOpType
AX = mybir.AxisListType

D = 2048
NCOL = 1536
NRES = 1408
NW = 2
WSZ = 4096
NEG = -1.0e30


class Sched:
    ENGS = ("pe", "act", "dve", "pool", "sp")

    def __init__(self, nc):
        self.nc = nc
        self.ops = []
        self.last_w = {}
        self.readers = {}
        self.bar = None
        self.last_on = {}
        self.dmas_since = []

    def op(self, eng, fn, reads=(), writes=(), dma=None, nobar=False):
        i = len(self.ops)
        deps = set()
        psr = [k for k in reads if isinstance(k, tuple) and k[0] == "ps"]
        if psr:
            reads = [k for k in reads if not (isinstance(k, tuple) and k[0] == "ps")]
            writes = list(writes) + psr
        for k in reads:
            j = self.last_w.get(k)
            if j is not None:
                deps.add(j)
        for k in writes:
            j = self.last_w.get(k)
            if j is not None:
                deps.add(j)
            for r in self.readers.get(k, ()):
                deps.add(r)
        if self.bar is not None and not nobar:
            deps.add(self.bar)
        deps.discard(i)
        self.ops.append(dict(eng=eng, fn=fn, deps=deps, dma=dma, needed=False, seq=None, dval=None))
        for k in reads:
            self.readers.setdefault(k, []).append(i)
        for k in writes:
            self.last_w[k] = i
            self.readers[k] = []
        if not nobar:
            if dma is not None:
                self.dmas_since.append(i)
            else:
                self.last_on[eng] = i
        return i

    def barrier(self):
        i = len(self.ops)
        deps = set(self.last_on.values()) | set(self.dmas_since)
        if self.bar is not None:
            deps.add(self.bar)
        self.ops.append(dict(eng="act", fn=self.barfn, deps=deps, dma=None, needed=False, seq=None, dval=None))
        self.bar = i
        self.last_on = {}
        self.dmas_since = []
        return i

    def emit(self, stack):
        nc = self.nc
        ops = self.ops
        for o in ops:
            for j in o["deps"]:
                pj = ops[j]
                if pj["dma"] is None and pj["eng"] == "pe" and o["eng"] == "pe" and o["dma"] is None and pj["fn"] is not None:
                    continue
                pj["needed"] = True
        esem = {e: stack.enter_context(nc.semaphore("s_" + e)) for e in self.ENGS}
        dsem = {}
        ecount = {e: 0 for e in self.ENGS}
        dcount = {}
        for o in ops:
            if o["dma"] is not None:
                if o["dma"] not in dsem:
                    dsem[o["dma"]] = stack.enter_context(nc.semaphore("d_%d" % len(dsem)))
                    dcount[o["dma"]] = 0
                dcount[o["dma"]] += 16
                o["dval"] = dcount[o["dma"]]
            elif o["needed"]:
                ecount[o["eng"]] += 1
                o["seq"] = ecount[o["eng"]]
        streams = {e: [] for e in self.ENGS}
        waited = {e: {} for e in self.ENGS}
        for i, o in enumerate(ops):
            e = o["eng"]
            ws = []
            for j in sorted(o["deps"]):
                pj = ops[j]
                if pj["dma"] is not None:
                    sem, val, key = dsem[pj["dma"]], pj["dval"], ("d", pj["dma"])
                else:
                    if pj["eng"] == "pe" and e == "pe" and o["dma"] is None and pj["fn"] is not None:
                        continue
                    sem, val, key = esem[pj["eng"]], pj["seq"], ("e", pj["eng"])
                if waited[e].get(key, 0) >= val:
                    continue
                waited[e][key] = val
                ws.append((sem, val))
            streams[e].append((ws, o))
        self.ecount = ecount

        def run(eng, items):
            for ws, o in items:
                for sem, val in ws:
                    eng.wait_ge(sem, val)
                if o["fn"] is None:
                    if o["needed"]:
                        eng.sem_inc(esem[o["eng"]], 1)
                    continue
                r = o["fn"](eng)
                if o["dma"] is not None:
                    r.then_inc(dsem[o["dma"]], 16)
                elif o["needed"]:
                    r.then_inc(esem[o["eng"]], 1)

        with nc.Block() as block:
            @block.sync
            def _(eng):
                run(eng, streams["sp"])

            @block.tensor
            def _(eng):
                run(eng, streams["pe"])

            @block.scalar
            def _(eng):
                run(eng, streams["act"])

            @block.vector
            def _(eng):
                run(eng, streams["dve"])

            @block.gpsimd
            def _(eng):
                run(eng, streams["pool"])


SEC = dict(q=0, k=512, v=640, su=768, sv=1280, cb=1792, cc=2304, cx=2816, ga=3328, gb=3840, gate=4352)


def rope_partner():
    p = np.zeros(64, np.int64)
    for d in range(64):
        p[d] = d + 16 if (d % 32) < 16 else d - 16
    return p


def in_chunks():
    part = rope_partner()
    ch = {}
    for j in range(4):
        base = SEC["q"] + j * 128
        ch["Q%d" % j] = base + np.arange(128)
        ch["Qs%d" % j] = base + np.concatenate([part, 64 + part])
    for g, nm in enumerate("AB"):
        base = SEC["k"] + g * 64
        ch["K" + nm] = base + np.concatenate([np.arange(64), np.arange(64)])
        ch["Ks" + nm] = base + np.concatenate([part, part])
    for j in range(4):
        ch["u%d" % j] = SEC["su"] + j * 128 + np.arange(128)
        ch["B%d" % j] = SEC["cb"] + j * 128 + np.arange(128)
        ch["C%d" % j] = SEC["cc"] + j * 128 + np.arange(128)
        ch["x%d" % j] = SEC["cx"] + j * 128 + np.arange(128)
        ch["a%d" % j] = SEC["ga"] + j * 128 + np.arange(128)
        ch["b%d" % j] = SEC["gb"] + j * 128 + np.arange(128)
    return ch


IN_BLOCKS = [("Q0", "Qs0"), ("Q1", "Qs1"), ("Q2", "Qs2"), ("Q3", "Qs3"), ("KA", "KsA"), ("KB", "KsB"),
             ("V",), ("u0", "u1"), ("u2", "u3"), ("SV0",), ("SV1",),
             ("B0", "C0"), ("x0", "B1"), ("C1", "x1"), ("B2", "C2"), ("x2", "B3"), ("C3", "x3"),
             ("a0", "b0"), ("a1", "b1"), ("a2", "b2"), ("a3", "b3")]


def pack_weights(inp, plan):
    nblk = len(plan)
    out = np.zeros((nblk, 128, WSZ), np.float32)
    ch = in_chunks()
    for bi, d in enumerate(plan):
        kind, l = d[0], d[1]
        if kind == "ada":
            aw = inp["ada_w"][l].reshape(16, 128, 6 * D)
            b = d[2]
            out[bi] = aw[:, :, b * 256:(b + 1) * 256].transpose(1, 0, 2).reshape(128, WSZ)
        elif kind == "in":
            win = inp["w_in"][l].reshape(16, 128, -1)
            blk = IN_BLOCKS[d[2]]
            if blk[0] == "V":
                cols = SEC["v"] + np.arange(128)
                out[bi, :, :2048] = win[:, :, cols].transpose(1, 0, 2).reshape(128, 2048)
            elif blk[0].startswith("SV"):
                h = int(blk[0][2])
                cols = SEC["sv"] + h * 256 + np.arange(256)
                out[bi] = win[:, :, cols].transpose(1, 0, 2).reshape(128, WSZ)
            else:
                cols = np.concatenate([ch[blk[0]], ch[blk[1]]])
                out[bi] = win[:, :, cols].transpose(1, 0, 2).reshape(128, WSZ)
        elif kind == "g":
            win = inp["w_in"][l].reshape(16, 128, -1)
            dc, half = d[2], d[3]
            cols = np.concatenate([SEC["gate"] + gi * D + dc * 128 + np.arange(128) for gi in (2 * half, 2 * half + 1)])
            out[bi] = win[:, :, cols].transpose(1, 0, 2).reshape(128, WSZ)
        elif kind == "wb":
            wbr = inp["w_branch"][l].reshape(4, 4, 128, D)
            dc = d[2]
            out[bi, :, :2048] = wbr[:, :, :, dc * 128:(dc + 1) * 128].transpose(2, 0, 1, 3).reshape(128, 2048)
        elif kind == "wo":
            wo = inp["w_out"][l].reshape(16, 128, D)
            pr = d[2]
            out[bi] = wo[:, :, pr * 256:(pr + 1) * 256].transpose(1, 0, 2).reshape(128, WSZ)
        elif kind == "up":
            e, fc = d[2], d[3]
            wu = inp["exp_w_up"][l][e].reshape(16, 128, 2, 1024)
            out[bi] = wu[:, :, :, fc * 128:(fc + 1) * 128].transpose(1, 0, 2, 3).reshape(128, WSZ)
        elif kind == "dn":
            e, db = d[2], d[3]
            wd = inp["exp_w_down"][l][e].reshape(8, 128, D)
            out[bi] = wd[:, :, db * 512:(db + 1) * 512].transpose(1, 0, 2).reshape(128, WSZ)
    return out.reshape(nblk * 128, WSZ)


def vec_layout():
    off = {}
    o = 0
    for l in range(2):
        for nm, n in (("adab", 96), ("n1g", 16), ("n2g", 16), ("sink", 4), ("cdw", 124), ("cdb", 4),
                      ("clg", 4), ("clb", 4), ("scw", 12)):
            off[(nm, l)] = o
            o += n
    off[("fg", 0)] = o
    o += 16
    return off, o


VOFF, NVEC = vec_layout()

TCOL = [128 * t for t in range(9)] + [1408]
CTXC = [1152, 1280]
SEGS = [(0, 1152, "x"), (1152, 1408, "c"), (1408, 1536, "x")]
CPOS = [(0, 1152, 16), (1152, 1408, 1312), (1408, 1536, 1168)]
CONVL = 1584


def segs_of(c0, n):
    r = []
    for a, b, kind in SEGS:
        lo, hi = max(a, c0), min(b, c0 + n)
        if lo < hi:
            r.append((lo, hi - lo, kind))
    return r


def pieces_of(c0, n):
    r = []
    for a, b, pos in CPOS:
        lo, hi = max(a, c0), min(b, c0 + n)
        if lo < hi:
            r.append((lo, hi - lo, pos + lo - a))
    return r


NBLK = 634


def build(stop_after=None, nblk=NBLK):
    nc = bass.Bass("TRN2", target_bir_lowering=False)
    xin = nc.dram_tensor("xin", [NCOL, D], F32, kind="ExternalInput").ap()
    wts = nc.dram_tensor("wts", [nblk * 128, WSZ], F32, kind="ExternalInput").ap()
    rope_d = nc.dram_tensor("rope", [128, 2 * NCOL], F32, kind="ExternalInput").ap()
    mask_d = nc.dram_tensor("masks", [128, 1024], F32, kind="ExternalInput").ap()
    cin_d = nc.dram_tensor("cin", [128, 32], F32, kind="ExternalInput").ap()
    vecs_d = nc.dram_tensor("vecs", [128, NVEC], F32, kind="ExternalInput").ap()
    rowv_d = nc.dram_tensor("rowv", [2, 1536], F32, kind="ExternalInput").ap()
    sguw_d = nc.dram_tensor("sguw", [2 * 128, 512], F32, kind="ExternalInput").ap()
    rw_d = nc.dram_tensor("rw", [128, 256], F32, kind="ExternalInput").ap()
    rb_d = nc.dram_tensor("rb", [1, 16], F32, kind="ExternalInput").ap()
    out_d = nc.dram_tensor("out", [1024, D], F32, kind="ExternalOutput").ap()
    dbg_d = nc.dram_tensor("dbg", [128, 16 * NRES], F32, kind="ExternalOutput").ap() if stop_after is not None else None
    xsp = nc.dram_tensor("xsp", [128, 16 * NRES], F32, kind="Internal").ap()
    ctd = nc.dram_tensor("ctd", [16, NRES], F32, kind="Internal").ap()

    st = ExitStack()
    with st:
        S = Sched(nc)
        S.barfn = lambda e: e.activation(out=sml[:, 0:1], in_=sml[:, 1:2], func=AF.Copy)
        A = st.enter_context(nc.sbuf_tensor("A", [128, 90112], U8))
        C = st.enter_context(nc.sbuf_tensor("C", [128, 45056], U8))
        hT = st.enter_context(nc.sbuf_tensor("hT", [128, 16, NCOL], BF16))
        wsl = [st.enter_context(nc.sbuf_tensor("w%d" % i, [128, WSZ], BF16)) for i in range(NW)]
        vecs = st.enter_context(nc.sbuf_tensor("vecs_sb", [128, NVEC], F32))
        modp = st.enter_context(nc.sbuf_tensor("modp", [128, 4, 96], F32))
        gsv = st.enter_context(nc.sbuf_tensor("gsv", [128, 8, 16], F32))
        ident = st.enter_context(nc.sbuf_tensor("ident", [128, 128], F32))
        ones = st.enter_context(nc.sbuf_tensor("ones", [128, 128], F32))
        oneslh = st.enter_context(nc.sbuf_tensor("oneslh", [128, 2, 128], BF16))
        identb = st.enter_context(nc.sbuf_tensor("identb", [128, 128], BF16))
        cbf = st.enter_context(nc.sbuf_tensor("cbf", [128, 16, 2], BF16))
        cf32 = st.enter_context(nc.sbuf_tensor("cf32", [128, 32], F32))
        rw = st.enter_context(nc.sbuf_tensor("rw_sb", [128, 16, 16], F32))
        rbb = st.enter_context(nc.sbuf_tensor("rbb", [128, 16], F32))
        esk = st.enter_context(nc.sbuf_tensor("esk", [128, 4], F32))
        sml = st.enter_context(nc.sbuf_tensor("sml", [128, 32], F32))
        ps = [st.enter_context(nc.psum_tensor("ps%d" % i, [128, 512], F32)) for i in range(8)]

        def view(ar, off, dt, shape):
            nel = int(np.prod(shape))
            sz = 4 if dt == F32 else 2
            v = ar[:, off:off + nel * sz].bitcast(dt)
            if len(shape) == 2:
                v = v.rearrange("p (a b) -> p a b", a=shape[0])
            elif len(shape) == 3:
                v = v.rearrange("p (a b c) -> p a b c", a=shape[0], b=shape[1])
            elif len(shape) == 4:
                v = v.rearrange("p (a b c d) -> p a b c d", a=shape[0], b=shape[1], c=shape[2])
            return v

        xT = view(A, 0, F32, [16, NRES])
        ybr = view(A, 0, BF16, [16, NRES])
        AU = 45056
        merged = view(C, 0, BF16, [16, NRES])

        bank_rr = [0]

        def nb():
            b = bank_rr[0]
            bank_rr[0] = (b + 1) % 7
            return b

        rings = {}

        def ring(name, n):
            i = rings.get(name, 0)
            rings[name] = i + 1
            return i % n

        wstate = dict(next=0)

        plan = []

        def wload(desc):
            b = wstate["next"]
            plan.append(desc)
            wstate["next"] = b + 1
            s = b % NW
            S.op("pool", lambda e, b=b, s=s: e.dma_start(out=wsl[s][:], in_=wts[b * 128:(b + 1) * 128, :]),
                 writes=[("w", s)], dma="w%d" % s, nobar=True)
            return s

        def wkey(s):
            return ("w", s)

        S.op("sp", lambda e: e.dma_start(out=vecs[:], in_=vecs_d), writes=["vecs"], dma="k1")
        S.op("sp", lambda e: e.dma_start(out=cf32[:], in_=cin_d), writes=["cf32"], dma="k2")
        S.op("sp", lambda e: e.dma_start(out=rw[:].rearrange("p k e -> p (k e)"), in_=rw_d), writes=["rw"], dma="k3")
        S.op("sp", lambda e: e.dma_start(out=rbb[:], in_=rb_d[0, :].partition_broadcast(128)), writes=["rbb"], dma="k4")
        S.op("dve", lambda e: e.memset(sml[:], 0.0), writes=["sml"])
        S.op("dve", lambda e: e.memset(ones[:], 1.0), writes=["ones"])
        S.op("dve", lambda e: e.memset(ident[:], 0.0), writes=["ident"])
        S.op("pool", lambda e: e.affine_select(out=ident[:], in_=ones[:], pattern=[[-1, 128]], compare_op=ALU.is_equal,
                                               fill=0.0, base=0, channel_multiplier=1), reads=["ones"], writes=["ident"])
        S.op("dve", lambda e: e.tensor_copy(out=identb[:], in_=ident[:]), reads=["ident"], writes=["identb"])
        S.op("dve", lambda e: e.memset(oneslh[:], 0.0), writes=["oneslh"])
        S.op("dve", lambda e: e.memset(oneslh[:, 0, 0:64], 1.0), writes=["oneslh"])
        S.op("dve", lambda e: e.memset(oneslh[:, 1, 64:128], 1.0), writes=["oneslh"])
        S.op("act", lambda e: e.activation(out=cbf[:].rearrange("p k t -> p (k t)"), in_=cf32[:], func=AF.Silu),
             reads=["cf32"], writes=["cbf"])

        def V(nm, l, j, n=1):
            o = VOFF[(nm, l)] + j
            return vecs[:, o:o + n]

        stage = view(C, 0, F32, [2, D])
        X9 = view(C, 16384, F32, [16, 128])
        for i in range(12):
            sb = i % 2
            S.op("sp", lambda e, i=i, sb=sb: e.dma_start(out=stage[:, sb, :], in_=xin[i * 128:(i + 1) * 128, :]),
                 writes=[("stage", sb)], dma="st%d" % sb)
            for k4 in range(4):
                b = nb()

                def tr(e, sb=sb, k4=k4, b=b):
                    for kk in range(4):
                        k = k4 * 4 + kk
                        ins = e.transpose(out=ps[b][:, kk * 128:(kk + 1) * 128], in_=stage[:, sb, k * 128:(k + 1) * 128],
                                          identity=ident[:])
                    return ins
                S.op("pe", tr, reads=[("stage", sb), "ident"], writes=[("ps", b)])
                if i < 11:
                    dst = xT[:, k4 * 4:(k4 + 1) * 4, i * 128:(i + 1) * 128]
                    wk = [("xT", k4 * 4 + kk, i // 4 if i < 8 else 2) for kk in range(4)]
                else:
                    dst = X9[:, k4 * 4:(k4 + 1) * 4, :]
                    wk = ["X9"]
                S.op("act" if k4 % 2 == 0 else "dve",
                     (lambda e, dst=dst, b=b: e.activation(out=dst, in_=ps[b][:].rearrange("p (a c) -> p a c", a=4), func=AF.Copy))
                     if k4 % 2 == 0 else
                     (lambda e, dst=dst, b=b: e.tensor_copy(out=dst, in_=ps[b][:].rearrange("p (a c) -> p a c", a=4))),
                     reads=[("ps", b)], writes=wk)

        def xkeys(k, c0, n):
            return [("xT", k, g) for g in range(3) if c0 < min(512 * (g + 1), NRES) and c0 + n > 512 * g]

        GOUT = {0: [(0, 512), (512, 512), (1024, 384)], 1: [(0, 512), (512, 512)]}
        GIN = {0: [(0, 512), (512, 512), (1024, 512)], 1: [(0, 512), (512, 512), (1024, 384)]}

        def ada_block(l, b):
            s = wload(("ada", l, b))
            for cc in range(2):
                chunk = b * 2 + cc

                def mm(e, s=s, cc=cc, chunk=chunk):
                    for k in range(16):
                        ins = e.matmul(ps[7][:, chunk * 2:chunk * 2 + 2], lhsT=wsl[s][:, k * 256 + cc * 128:k * 256 + cc * 128 + 128],
                                       rhs=cbf[:, k, :], start=(k == 0), stop=(k == 15))
                    return ins
                S.op("pe", mm, reads=[wkey(s), "cbf"], writes=[("ps", 7)])

        def ada_finish(l, secs):
            pv = ps[7][:, 0:192].rearrange("p (c t) -> p c t", t=2)
            for sec in secs:
                for t in range(2):
                    S.op("dve", lambda e, t=t, sec=sec: e.tensor_tensor(out=modp[:, l * 2 + t, sec * 16:(sec + 1) * 16], in0=pv[:, sec * 16:(sec + 1) * 16, t],
                                                                        in1=V("adab", l, sec * 16, 16), op=ALU.add),
                         reads=[("ps", 7), "vecs"], writes=[("modp", l)])
            for ni, (gn, sec) in enumerate((("n1g", 1), ("n2g", 4))):
                if sec not in secs:
                    continue
                for t in range(2):
                    S.op("dve", lambda e, ni=ni, gn=gn, sec=sec, t=t: e.scalar_tensor_tensor(
                        out=gsv[:, l * 4 + ni * 2 + t, :], in0=modp[:, l * 2 + t, sec * 16:(sec + 1) * 16], scalar=1.0, in1=V(gn, l, 0, 16),
                        op0=ALU.add, op1=ALU.mult), reads=[("modp", l), "vecs"], writes=[("gsv", l)])

        ada_q = {0: list(range(48)), 1: list(range(48))}

        def ada_some(l, n):
            for _ in range(n):
                if ada_q[l]:
                    ada_block(l, ada_q[l].pop(0))

        cur = dict(l=0)

        def modcol(t, sec, k, ll):
            return modp[:, ll * 2 + t, sec * 16 + k:sec * 16 + k + 1]

        rstd = view(C, 24576, F32, [NCOL])
        sqt = view(C, 30720, F32, [2, 512])
        ntt = view(C, 34816, F32, [2, 512])
        hx32 = view(C, 38912, F32, [2, 512])

        def norm_phase(groups, src, srckeys, ni, shsec, router=None, final=False):
            ll = cur["l"]
            for (c0, n) in groups:
                b = nb()
                for k in range(16):
                    r = ring("sqt", 2)
                    if k % 2 == 0:
                        S.op("act", lambda e, k=k, r=r, c0=c0, n=n: e.activation(out=sqt[:, r, 0:n], in_=src(k, c0, n), func=AF.Square),
                             reads=srckeys(k, c0, n), writes=[("sqt", r)])
                    else:
                        S.op("dve", lambda e, k=k, r=r, c0=c0, n=n: e.tensor_tensor(out=sqt[:, r, 0:n], in0=src(k, c0, n), in1=src(k, c0, n), op=ALU.mult),
                             reads=srckeys(k, c0, n), writes=[("sqt", r)])
                    S.op("pe", lambda e, k=k, r=r, b=b, n=n: e.matmul(ps[b][:, 0:n], lhsT=ones[:], rhs=sqt[:, r, 0:n],
                                                                      start=(k == 0), stop=(k == 15)),
                         reads=[("sqt", r), "ones"], writes=[("ps", b)])
                S.op("act", lambda e, b=b, c0=c0, n=n: e.activation(out=rstd[:, c0:c0 + n], in_=ps[b][:, 0:n], func=AF.Sqrt,
                                                                    bias=1e-6, scale=1.0 / D),
                     reads=[("ps", b)], writes=[("rstd", c0)])
                S.op("dve", lambda e, c0=c0, n=n: e.reciprocal(out=rstd[:, c0:c0 + n], in_=rstd[:, c0:c0 + n]),
                     reads=[("rstd", c0)], writes=[("rstd", c0)])
                if final:
                    continue
                rb = nb() if router is not None else None
                for k in range(16):
                    r = ring("ntt", 2)
                    S.op("dve", lambda e, k=k, r=r, c0=c0, n=n: e.tensor_tensor(out=ntt[:, r, 0:n], in0=src(k, c0, n), in1=rstd[:, c0:c0 + n], op=ALU.mult),
                         reads=srckeys(k, c0, n) + [("rstd", c0)], writes=[("ntt", r)])
                    if router is None:
                        for (s0, sn, kind) in segs_of(c0, n):
                            t = 0 if kind == "x" else 1
                            S.op("act", lambda e, k=k, r=r, s0=s0, sn=sn, t=t, c0=c0: e.activation(
                                out=hT[:, k, s0:s0 + sn], in_=ntt[:, r, s0 - c0:s0 - c0 + sn], func=AF.Identity,
                                scale=gsv[:, ll * 4 + ni * 2 + t, k:k + 1], bias=modcol(t, shsec, k, ll)),
                                reads=[("ntt", r), ("gsv", 0), ("gsv", 1), ("modp", 0), ("modp", 1)], writes=[("hT", k, min(c0, 1024))])
                    else:
                        r2 = ring("hx32", 2)
                        for (s0, sn, kind) in segs_of(c0, n):
                            t = 0 if kind == "x" else 1
                            S.op("act", lambda e, k=k, r=r, r2=r2, s0=s0, sn=sn, t=t, c0=c0: e.activation(
                                out=hx32[:, r2, s0 - c0:s0 - c0 + sn], in_=ntt[:, r, s0 - c0:s0 - c0 + sn], func=AF.Identity,
                                scale=gsv[:, ll * 4 + ni * 2 + t, k:k + 1], bias=modcol(t, shsec, k, ll)),
                                reads=[("ntt", r), ("gsv", 0), ("gsv", 1), ("modp", 0), ("modp", 1)], writes=[("hx32", r2)])
                        if k % 2 == 0:
                            S.op("pool", lambda e, k=k, r2=r2, c0=c0, n=n: e.tensor_copy(out=hT[:, k, c0:c0 + n], in_=hx32[:, r2, 0:n]),
                                 reads=[("hx32", r2)], writes=[("hT", k, min(c0, 1024))])
                        else:
                            S.op("act", lambda e, k=k, r2=r2, c0=c0, n=n: e.activation(out=hT[:, k, c0:c0 + n], in_=hx32[:, r2, 0:n], func=AF.Copy),
                                 reads=[("hx32", r2)], writes=[("hT", k, min(c0, 1024))])
                        S.op("pe", lambda e, k=k, r2=r2, rb=rb, n=n: e.matmul(ps[rb][0:16, 0:n], lhsT=rw[:, k, :], rhs=hx32[:, r2, 0:n],
                                                                           start=(k == 0), stop=(k == 15)),
                             reads=[("hx32", r2), "rw"], writes=[("ps", rb)])
                if router is not None:
                    router(c0, n, rb)

        def hkeys(c0):
            return [("hT", k, c0) for k in range(16)]

        def proj(s, cc, ncc, c0, n, b):
            def mm(e):
                for k in range(16):
                    o = (k * ncc + cc) * 128
                    ins = e.matmul(ps[b][:, 0:n], lhsT=wsl[s][:, o:o + 128], rhs=hT[:, k, c0:c0 + n], start=(k == 0), stop=(k == 15))
                return ins
            S.op("pe", mm, reads=[wkey(s)] + hkeys(c0), writes=[("ps", b)])

        for l in range(2):
            gin, gout = GIN[l], GOUT[l]
            if stop_after == "init":
                break
            cur["l"] = l
            if l == 0:
                ada_some(0, 16)
                ada_finish(0, [0, 1])
            else:
                ada_some(1, 48)
                ada_finish(1, [0, 1, 2, 3, 4, 5])
            if l == 0:
                def src0(k, c0, n):
                    return X9[:, k, c0 - 1408:c0 - 1408 + n] if c0 >= 1408 else xT[:, k, c0:c0 + n]

                def sk0(k, c0, n):
                    return ["X9"] if c0 >= 1408 else xkeys(k, c0, n)
                norm_phase([(0, 512), (512, 512), (1024, 384), (1408, 128)], src0, sk0, 0, 0)
            else:
                norm_phase([(0, 512), (512, 512), (1024, 384)], lambda k, c0, n: xT[:, k, c0:c0 + n], xkeys, 0, 0)
            S.op("sp", lambda e: e.dma_start(out=xsp, in_=A[:, 0:16 * NRES * 4].bitcast(F32)),
                 reads=[("xT", k, g) for k in range(16) for g in range(3)], writes=["xsp"], dma="xsp")
            S.barrier()

            if stop_after == "n1":
                break
            QT = view(A, AU, BF16, [4, NCOL])
            KTZ = view(A, AU + 12288, BF16, [2, 2, NCOL])
            VLH = view(A, AU + 24576, BF16, [12, 2, 2, 128])
            PT = view(C, 26624, BF16, [2, 5, 512])
            ropeT = view(C, 0, F32, [2, NCOL])
            maskT = view(C, 12288, F32, [2, 512])
            rt1 = view(C, 16384, F32, [2, 512])
            rt2 = view(C, 20480, F32, [2, 512])
            rdt = view(C, 24576, F32, [2, 256])
            S.op("sp", lambda e: e.dma_start(out=ropeT[:].rearrange("p a c -> p (a c)"), in_=rope_d), writes=["rope"], dma="k5")
            S.op("sp", lambda e: e.dma_start(out=maskT[:].rearrange("p a c -> p (a c)"), in_=mask_d), writes=["mask"], dma="k6")
            S.op("act", lambda e, l=l: e.activation(out=esk[:], in_=V("sink", l, 0, 4), func=AF.Exp), reads=["vecs"], writes=["esk"])
            S.op("dve", lambda e: e.memset(VLH[:].rearrange("p a b c d -> p (a b c d)"), 0.0), writes=["VLH"])
            S.op("dve", lambda e: e.memset(KTZ[:].rearrange("p a b c -> p (a b c)"), 0.0), writes=["KTZ"])
            for bi in range(6):
                s = wload(("in", l, bi))
                for (c0, n) in (gin[:2] if (l == 1 and bi < 4) else gin):
                    b1, b2 = nb(), nb()
                    proj(s, 0, 2, c0, n, b1)
                    proj(s, 1, 2, c0, n, b2)
                    r = ring("rt", 2)
                    S.op("dve", lambda e, b1=b1, r=r, c0=c0, n=n: e.tensor_tensor(out=rt1[:, r, 0:n], in0=ps[b1][:, 0:n], in1=ropeT[:, 0, c0:c0 + n], op=ALU.mult),
                         reads=[("ps", b1), "rope"], writes=[("rt1", r)])
                    S.op("dve", lambda e, b2=b2, r=r, c0=c0, n=n: e.tensor_tensor(out=rt2[:, r, 0:n], in0=ps[b2][:, 0:n], in1=ropeT[:, 1, c0:c0 + n], op=ALU.mult),
                         reads=[("ps", b2), "rope"], writes=[("rt2", r)])
                    if bi < 4:
                        S.op("dve", lambda e, bi=bi, c0=c0, r=r, n=n: e.tensor_tensor(out=QT[:, bi, c0:c0 + n], in0=rt1[:, r, 0:n], in1=rt2[:, r, 0:n], op=ALU.add),
                             reads=[("rt1", r), ("rt2", r)], writes=[("QK", bi, c0)])
                    else:
                        for hh in range(2):
                            S.op("dve", lambda e, bi=bi, c0=c0, r=r, n=n, hh=hh: e.tensor_tensor(
                                out=KTZ[hh * 64:hh * 64 + 64, bi - 4, hh, c0:c0 + n], in0=rt1[hh * 64:hh * 64 + 64, r, 0:n], in1=rt2[hh * 64:hh * 64 + 64, r, 0:n], op=ALU.add),
                                reads=[("rt1", r), ("rt2", r), "KTZ"], writes=[("QK", bi, c0, hh)])
            s = wload(("in", l, 6))
            for tb in range(3):
                b = nb()

                def mmv(e, s=s, tb=tb, b=b):
                    for tt in range(4):
                        col = (tb * 4 + tt) * 128
                        for k in range(16):
                            ins = e.matmul(ps[b][:, tt * 128:(tt + 1) * 128], lhsT=hT[:, k, col:col + 128], rhs=wsl[s][:, k * 128:(k + 1) * 128],
                                           start=(k == 0), stop=(k == 15))
                    return ins
                S.op("pe", mmv, reads=[wkey(s)] + hkeys(0) + hkeys(512) + hkeys(1024), writes=[("ps", b)])
                pvv = ps[b][:].rearrange("p (t c) -> p t c", t=4)
                for g in range(2):
                    for var in range(2):
                        S.op("act" if var == 0 else "dve",
                             (lambda e, tb=tb, g=g, var=var, pvv=pvv: e.activation(out=VLH[:, tb * 4:(tb + 1) * 4, g, var, var * 64:var * 64 + 64],
                                                                                   in_=pvv[:, :, g * 64:(g + 1) * 64], func=AF.Copy))
                             if var == 0 else
                             (lambda e, tb=tb, g=g, var=var, pvv=pvv: e.tensor_copy(out=VLH[:, tb * 4:(tb + 1) * 4, g, var, var * 64:var * 64 + 64],
                                                                                    in_=pvv[:, :, g * 64:(g + 1) * 64])),
                             reads=[("ps", b), "VLH"], writes=[("VLHw", tb, g, var)])
            vlh_keys = [("VLHw", tb, g, var) for tb in range(3) for g in range(2) for var in range(2)]
            qk_keys = [("QK", bi, c0) for bi in range(4) for (c0, n) in gin] + [("QK", bi, c0, hh) for bi in (4, 5) for (c0, n) in gin for hh in range(2)]
            if stop_after == "att1":
                break
            qtiles = []
            for t in range(9 if l == 0 else 8):
                kts = []
                if t >= 1:
                    kts.append((TCOL[t - 1], 0))
                kts.append((TCOL[t], None))
                kts.append((TCOL[t + 1], 1))
                kts += [(CTXC[0], None), (CTXC[1], None)]
                qtiles.append((TCOL[t], kts))
            if l == 0:
                for cq in CTXC:
                    qtiles.append((cq, [(CTXC[0], None), (CTXC[1], None)]))
            def att_stage_a(qc, kts, g, pb):
                for ki, (kc, mk) in enumerate(kts):
                    b = nb()

                    def mms(e, g=g, kc=kc, qc=qc, b=b):
                        for hh in range(2):
                            for cq in range(2):
                                ins = e.matmul(ps[b][:, hh * 256 + cq * 128:hh * 256 + cq * 128 + 128], lhsT=KTZ[:, g, hh, kc:kc + 128],
                                               rhs=QT[:, 2 * g + cq, qc:qc + 128], start=True, stop=True)
                        return ins
                    S.op("pe", mms, reads=qk_keys, writes=[("ps", b)])
                    S.op("act", lambda e, pb=pb, ki=ki, b=b: e.activation(out=PT[:, pb, ki, :], in_=ps[b][:], func=AF.Exp, scale=0.125),
                         reads=[("ps", b)], writes=[("PT", pb, ki)])
                    if mk is not None:
                        S.op("dve", lambda e, pb=pb, ki=ki, mk=mk: e.tensor_tensor(out=PT[:, pb, ki, :], in0=PT[:, pb, ki, :], in1=maskT[:, mk, :], op=ALU.mult),
                             reads=[("PT", pb, ki), "mask"], writes=[("PT", pb, ki)])

            def att_stage_b(qc, kts, g, pb):
                bo, bd = nb(), nb()
                nk = len(kts)

                def mmo(e):
                    for ki, (kc, mk) in enumerate(kts):
                        ct = kc // 128
                        e.matmul(ps[bo][:, 0:256], lhsT=VLH[:, ct, g, 0, :], rhs=PT[:, pb, ki, 0:256], start=(ki == 0), stop=False)
                        ins = e.matmul(ps[bo][:, 0:256], lhsT=VLH[:, ct, g, 1, :], rhs=PT[:, pb, ki, 256:512], start=False, stop=(ki == nk - 1))
                    return ins

                def mmd(e):
                    for ki in range(nk):
                        e.matmul(ps[bd][:, 0:256], lhsT=oneslh[:, 0, :], rhs=PT[:, pb, ki, 0:256], start=(ki == 0), stop=False)
                        ins = e.matmul(ps[bd][:, 0:256], lhsT=oneslh[:, 1, :], rhs=PT[:, pb, ki, 256:512], start=False, stop=(ki == nk - 1))
                    return ins
                ptk = [("PT", pb, ki) for ki in range(nk)]
                S.op("pe", mmo, reads=ptk + vlh_keys, writes=[("ps", bo)])
                S.op("pe", mmd, reads=ptk + ["oneslh"], writes=[("ps", bd)])
                rr = ring("rdt", 2)
                for cc in range(2):
                    S.op("dve", lambda e, cc=cc: e.tensor_single_scalar(out=rdt[:, rr, cc * 128:(cc + 1) * 128], in_=ps[bd][:, cc * 128:(cc + 1) * 128],
                                                                        scalar=esk[:, 2 * g + cc:2 * g + cc + 1], op=ALU.add),
                         reads=[("ps", bd), "esk"], writes=[("rdt", rr, cc)])
                S.op("dve", lambda e: e.reciprocal(out=rdt[:, rr, :], in_=rdt[:, rr, :]),
                     reads=[("rdt", rr, 0), ("rdt", rr, 1)], writes=[("rdt", rr, 0), ("rdt", rr, 1)])
                S.op("dve", lambda e: e.tensor_tensor(
                    out=ybr[:, 2 * g:2 * g + 2, qc:qc + 128], in0=ps[bo][:, 0:256].rearrange("p (a c) -> p a c", a=2),
                    in1=rdt[:, rr, :].rearrange("p (a c) -> p a c", a=2), op=ALU.mult),
                    reads=[("ps", bo), ("rdt", rr, 0), ("rdt", rr, 1)], writes=[("ybr", 0, qc)])

            pairs = [(qc, kts, g) for (qc, kts) in qtiles for g in range(2)]
            for i, (qc, kts, g) in enumerate(pairs):
                if l == 0 and g == 0:
                    ada_some(0, 3)
                att_stage_a(qc, kts, g, i % 2)
                if i >= 1:
                    pq, pk, pg = pairs[i - 1]
                    att_stage_b(pq, pk, pg, (i - 1) % 2)
            pq, pk, pg = pairs[-1]
            att_stage_b(pq, pk, pg, (len(pairs) - 1) % 2)
            if l == 0:
                ada_some(0, 48)
                ada_finish(0, [2, 3, 4, 5])
            S.barrier()

            if stop_after == "att":
                break
            UG = view(A, AU, BF16, [4, NCOL])
            gsq = view(A, AU + 12288, F32, [2, 512])
            gin_ = view(A, AU + 16384, F32, [2, 512])
            gsg = view(A, AU + 20480, F32, [2, 512])
            cen = view(A, AU + 24576, F32, [2, 512])
            sqv = view(A, AU + 28672, F32, [2, 512])
            VG = view(C, 0, F32, [12, 512])
            VTM = view(C, 24576, BF16, [12, 512])
            wsT = view(C, 36864, BF16, [4, 128])
            bsb = view(C, 37888, F32, [4, 128])
            lngb = view(C, 39936, F32, [512])
            lnbb = view(C, 41984, F32, [512])
            sgt = view(C, 44032, F32, [2, 128])
            wsTf = view(A, AU + 32768, F32, [512])
            S.op("sp", lambda e, l=l: e.dma_start(out=wsTf[:], in_=sguw_d[l * 128:(l + 1) * 128, :]), writes=["wsTf"], dma="k7")
            S.op("act", lambda e: e.activation(out=wsT[:].rearrange("p g c -> p (g c)"), in_=wsTf[:], func=AF.Copy), reads=["wsTf"], writes=["wsT"])
            S.op("sp", lambda e, l=l: e.dma_start(out=lngb[:], in_=rowv_d[l, 0:512].partition_broadcast(128)), writes=["lngb"], dma="k8")
            S.op("sp", lambda e, l=l: e.dma_start(out=lnbb[:], in_=rowv_d[l, 512:1024].partition_broadcast(128)), writes=["lnbb"], dma="k9")
            S.op("sp", lambda e, l=l: e.dma_start(out=bsb[:].rearrange("p g c -> p (g c)"), in_=rowv_d[l, 1024:1536].partition_broadcast(128)), writes=["bsb"], dma="k10")

            def gelu_ops(src_ap, src_keys, n, dst_ap, dst_keys):
                r = ring("gelu", 2)
                S.op("act", lambda e: e.activation(out=gsq[:, r, 0:n], in_=src_ap, func=AF.Square), reads=src_keys, writes=[("gsq", r)])
                S.op("dve", lambda e: e.tensor_scalar(out=gin_[:, r, 0:n], in0=gsq[:, r, 0:n], scalar1=0.044715, scalar2=1.0, op0=ALU.mult, op1=ALU.add),
                     reads=[("gsq", r)], writes=[("gin", r)])
                S.op("dve", lambda e: e.tensor_tensor(out=gin_[:, r, 0:n], in0=gin_[:, r, 0:n], in1=src_ap, op=ALU.mult),
                     reads=[("gin", r)] + src_keys, writes=[("gin", r)])
                S.op("act", lambda e: e.activation(out=gsg[:, r, 0:n], in_=gin_[:, r, 0:n], func=AF.Sigmoid, scale=1.5957691216057308),
                     reads=[("gin", r)], writes=[("gsg", r)])
                S.op("dve", lambda e: e.tensor_tensor(out=dst_ap, in0=gsg[:, r, 0:n], in1=src_ap, op=ALU.mult),
                     reads=[("gsg", r)] + src_keys, writes=dst_keys)

            for bi in range(2):
                s = wload(("in", l, 7 + bi))
                for cc in range(2):
                    j = bi * 2 + cc
                    for (c0, n) in (gin[:2] if l == 1 else gin):
                        b = nb()
                        proj(s, cc, 2, c0, n, b)
                        gelu_ops(ps[b][:, 0:n], [("ps", b)], n, UG[:, j, c0:c0 + n], [("UG", j, c0)])
            ntile_v = 11 if l == 0 else 8
            vcols = [128 * i for i in range(11)] if l == 0 else [128 * i for i in range(8)]
            for hb in range(2):
                s = wload(("in", l, 9 + hb))
                for ti, col in enumerate(vcols):
                    b = nb()

                    def mmsv(e, s=s, col=col, b=b):
                        for k in range(16):
                            ins = e.matmul(ps[b][:, 0:256], lhsT=hT[:, k, col:col + 128], rhs=wsl[s][:, k * 256:(k + 1) * 256],
                                           start=(k == 0), stop=(k == 15))
                        return ins
                    S.op("pe", mmsv, reads=[wkey(s)] + hkeys((col // 512) * 512), writes=[("ps", b)])
                    gelu_ops(ps[b][:, 0:256], [("ps", b)], 256, VG[:, ti, hb * 256:(hb + 1) * 256], [("VG", ti, hb)])
            for ti, col in enumerate(vcols):
                vk = [("VG", ti, 0), ("VG", ti, 1)]
                r = ring("sgt", 2)
                S.op("dve", lambda e, ti=ti, r=r: e.tensor_reduce(out=sgt[:, r, 0:1], in_=VG[:, ti, :], axis=AX.X, op=ALU.add), reads=vk, writes=[("sgt", r)])
                S.op("dve", lambda e, r=r: e.tensor_single_scalar(out=sgt[:, r, 1:2], in_=sgt[:, r, 0:1], scalar=1.0 / 512, op=ALU.mult),
                     reads=[("sgt", r)], writes=[("sgt", r)])
                S.op("dve", lambda e, ti=ti, r=r: e.tensor_single_scalar(out=cen[:, r, :], in_=VG[:, ti, :], scalar=sgt[:, r, 1:2], op=ALU.subtract),
                     reads=vk + [("sgt", r)], writes=[("cen", r)])
                S.op("dve", lambda e, r=r: e.tensor_tensor(out=sqv[:, r, :], in0=cen[:, r, :], in1=cen[:, r, :], op=ALU.mult), reads=[("cen", r)], writes=[("sqv", r)])
                S.op("dve", lambda e, r=r: e.tensor_reduce(out=sgt[:, r, 2:3], in_=sqv[:, r, :], axis=AX.X, op=ALU.add), reads=[("sqv", r), ("sgt", r)], writes=[("sgt", r)])
                S.op("act", lambda e, r=r: e.activation(out=sgt[:, r, 3:4], in_=sgt[:, r, 2:3], func=AF.Sqrt, bias=1e-5, scale=1.0 / 512), reads=[("sgt", r)], writes=[("sgt", r)])
                S.op("dve", lambda e, r=r: e.reciprocal(out=sgt[:, r, 4:5], in_=sgt[:, r, 3:4]), reads=[("sgt", r)], writes=[("sgt", r)])
                S.op("dve", lambda e, r=r: e.scalar_tensor_tensor(out=cen[:, r, :], in0=cen[:, r, :], scalar=sgt[:, r, 4:5], in1=lngb[:], op0=ALU.mult, op1=ALU.mult),
                     reads=[("cen", r), ("sgt", r), "lngb"], writes=[("cen", r)])
                S.op("dve", lambda e, ti=ti, r=r: e.tensor_tensor(out=VTM[:, ti, :], in0=cen[:, r, :], in1=lnbb[:], op=ALU.add),
                     reads=[("cen", r), "lnbb"], writes=[("VTM", ti)])
                b = nb()

                def mmsp(e, ti=ti, b=b):
                    for g in range(4):
                        ins = e.matmul(ps[b][:, g * 128:(g + 1) * 128], lhsT=VTM[:, ti, g * 128:(g + 1) * 128], rhs=wsT[:, g, :], start=True, stop=True)
                    return ins
                S.op("pe", mmsp, reads=[("VTM", ti), "wsT"], writes=[("ps", b)])
                r2 = ring("spt", 2)
                S.op("dve", lambda e, b=b, r2=r2: e.tensor_tensor(out=gsq[:, r2, :].rearrange("p (g c) -> p g c", g=4), in0=ps[b][:].rearrange("p (g c) -> p g c", g=4),
                                                                  in1=bsb[:], op=ALU.add), reads=[("ps", b), "bsb"], writes=[("gsq", r2)])
                S.op("dve", lambda e, r2=r2, col=col: e.tensor_tensor(out=ybr[:, 4:8, col:col + 128], in0=gsq[:, r2, :].rearrange("p (g c) -> p g c", g=4),
                                                                     in1=UG[:, :, col:col + 128], op=ALU.mult),
                     reads=[("gsq", r2)] + [("UG", j, (col // 512) * 512) for j in range(4)], writes=[("ybr", 1, col)])
            S.barrier()

            if stop_after == "sgu":
                break
            def sc_set(j):
                if j % 2 == 0:
                    return (view(C, 0, F32, [NCOL]), view(C, 6144, F32, [NCOL]), view(C, 12288, F32, [CONVL]), view(C, 18624, F32, [CONVL]))
                return (view(A, AU, F32, [NCOL]), view(A, AU + 6144, F32, [NCOL]), view(A, AU + 12288, F32, [CONVL]), view(A, AU + 18624, F32, [CONVL]))
            for j in range(2):
                zp = sc_set(j)[2]
                S.op("dve", lambda e, zp=zp: e.memset(zp[:], 0.0), writes=[("ZP", j)])
            sc_order = ["B0", "C0", "x0", "B1", "C1", "x1", "B2", "C2", "x2", "B3", "C3", "x3"]
            for ci, nm in enumerate(sc_order):
                if ci % 2 == 0:
                    if l == 0:
                        ada_some(1, 2)
                    s = wload(("in", l, 11 + ci // 2))
                cc = ci % 2
                j = int(nm[1])
                Bs, Cs, ZP, ACC = sc_set(j)
                for (c0, n) in (gin[:2] if (l == 1 and nm[0] == "B") else gin):
                    b = nb()
                    proj(s, cc, 2, c0, n, b)
                    if nm[0] == "B":
                        S.op("act", lambda e, Bs=Bs, b=b, c0=c0, n=n: e.activation(out=Bs[:, c0:c0 + n], in_=ps[b][:, 0:n], func=AF.Copy),
                             reads=[("ps", b)], writes=[("Bs", j % 2, c0)])
                    elif nm[0] == "C":
                        S.op("act", lambda e, Cs=Cs, b=b, c0=c0, n=n: e.activation(out=Cs[:, c0:c0 + n], in_=ps[b][:, 0:n], func=AF.Copy),
                             reads=[("ps", b)], writes=[("Cs", j % 2, c0)])
                    else:
                        for (q0, qn, pos) in pieces_of(c0, n):
                            S.op("dve", lambda e, ZP=ZP, Cs=Cs, b=b, c0=c0, q0=q0, qn=qn, pos=pos: e.tensor_tensor(
                                out=ZP[:, pos:pos + qn], in0=ps[b][:, q0 - c0:q0 - c0 + qn], in1=Cs[:, q0:q0 + qn], op=ALU.mult),
                                reads=[("ps", b), ("Cs", j % 2, c0), ("ZP", j % 2)], writes=[("ZPw", j % 2, c0)])
                if nm[0] == "x":
                    zk = [("ZPw", j % 2, c0) for (c0, n) in gin] + [("ZP", j % 2)]
                    L0, L1 = 16, CONVL - 16
                    S.op("dve", lambda e, ACC=ACC, ZP=ZP, j=j, l=l: e.tensor_single_scalar(out=ACC[:, L0:L1], in_=ZP[:, L0 - 1:L1 - 1], scalar=V("scw", l, j * 3 + 0), op=ALU.mult),
                         reads=zk + ["vecs"], writes=[("ACC", j % 2)])
                    for tap in (1, 2):
                        S.op("dve", lambda e, ACC=ACC, ZP=ZP, j=j, l=l, tap=tap: e.scalar_tensor_tensor(
                            out=ACC[:, L0:L1], in0=ZP[:, L0 - 1 + tap:L1 - 1 + tap], scalar=V("scw", l, j * 3 + tap), in1=ACC[:, L0:L1], op0=ALU.mult, op1=ALU.add),
                            reads=zk + [("ACC", j % 2), "vecs"], writes=[("ACC", j % 2)])
                    for (q0, qn, pos) in pieces_of(0, NRES if l == 0 else 1024):
                        S.op("dve", lambda e, ACC=ACC, Bs=Bs, j=j, q0=q0, qn=qn, pos=pos: e.tensor_tensor(
                            out=ybr[:, 8 + j, q0:q0 + qn], in0=ACC[:, pos:pos + qn], in1=Bs[:, q0:q0 + qn], op=ALU.mult),
                            reads=[("ACC", j % 2)] + [("Bs", j % 2, c0) for (c0, n) in gin], writes=[("ybr", 2, j, q0)])
            S.barrier()

            if stop_after == "sc":
                break
            ZC = view(A, AU, F32, [4, CONVL])
            ZP2 = view(C, 0, BF16, [2, CONVL])
            sgm = view(C, 6336, F32, [2, 512])
            lm = view(C, 10432, F32, [512])
            lmsq = view(C, 12480, F32, [512])
            lrs = view(C, 14528, F32, [512])
            lcen = view(C, 16576, F32, [2, 512])
            lsq = view(C, 20672, F32, [2, 512])
            DG = view(C, 24768, BF16, [2, 31, 128])
            lnp = [(16, 512, 0), (528, 512, 512)] + ([(1040, 128, 1024), (1312, 256, 1152)] if l == 0 else [])
            for r in range(2):
                S.op("dve", lambda e, r=r: e.memset(ZP2[:, r, :], 0.0), writes=[("ZP2", r)])
            for j in range(4):
                s = wload(("in", l, 17 + j))
                r = j % 2
                for tap in range(31):
                    if tap % 2 == 0:
                        S.op("act", lambda e, j=j, r=r, l=l, tap=tap: e.activation(out=DG[:, r, tap, :], in_=identb[:], func=AF.Identity, scale=V("cdw", l, j * 31 + tap)),
                             reads=["identb", "vecs"], writes=[("DG", r, tap)])
                    else:
                        S.op("dve", lambda e, j=j, r=r, l=l, tap=tap: e.tensor_single_scalar(out=DG[:, r, tap, :], in_=identb[:], scalar=V("cdw", l, j * 31 + tap), op=ALU.mult),
                             reads=["identb", "vecs"], writes=[("DG", r, tap)])
                for (c0, n) in gin:
                    ba, bb = nb(), nb()
                    proj(s, 0, 2, c0, n, ba)
                    proj(s, 1, 2, c0, n, bb)
                    rs = ring("sgm", 2)
                    S.op("act", lambda e, bb=bb, rs=rs, n=n: e.activation(out=sgm[:, rs, 0:n], in_=ps[bb][:, 0:n], func=AF.Sigmoid),
                         reads=[("ps", bb)], writes=[("sgm", rs)])
                    for (q0, qn, pos) in pieces_of(c0, n):
                        S.op("dve", lambda e, ba=ba, rs=rs, r=r, c0=c0, q0=q0, qn=qn, pos=pos: e.tensor_tensor(
                            out=ZP2[:, r, pos:pos + qn], in0=ps[ba][:, q0 - c0:q0 - c0 + qn], in1=sgm[:, rs, q0 - c0:q0 - c0 + qn], op=ALU.mult),
                            reads=[("ps", ba), ("sgm", rs), ("ZP2", r)], writes=[("ZP2w", r, c0)])
                if l == 0:
                    ada_some(1, 9)
                zk = [("ZP2w", r, c0) for (c0, n) in gin] + [("ZP2", r)]
                dgk = [("DG", r, tap) for tap in range(31)]
                for (pos, n, col) in lnp:
                    b = nb()

                    def mmc(e, r=r, pos=pos, n=n, b=b):
                        for tap in range(31):
                            ins = e.matmul(ps[b][:, 0:n], lhsT=DG[:, r, tap, :], rhs=ZP2[:, r, pos - 15 + tap:pos - 15 + tap + n], start=(tap == 0), stop=(tap == 30))
                        return ins
                    S.op("pe", mmc, reads=zk + dgk, writes=[("ps", b)])
                    S.op("act", lambda e, j=j, l=l, pos=pos, n=n, b=b: e.activation(out=ZC[:, j, pos:pos + n], in_=ps[b][:, 0:n], func=AF.Identity, bias=V("cdb", l, j), scale=1.0),
                         reads=[("ps", b), "vecs"], writes=[("ZC", j)])
            for (pos, n, col) in lnp:
                b1, b2 = nb(), nb()
                S.op("pe", lambda e, b1=b1, pos=pos, n=n: [e.matmul(ps[b1][:, 0:n], lhsT=ones[:], rhs=ZC[:, j, pos:pos + n], start=(j == 0), stop=(j == 3)) for j in range(4)][-1],
                     reads=[("ZC", j) for j in range(4)] + ["ones"], writes=[("ps", b1)])
                for j in range(4):
                    r = ring("lsq", 2)
                    S.op("act", lambda e, j=j, r=r, pos=pos, n=n: e.activation(out=lsq[:, r, 0:n], in_=ZC[:, j, pos:pos + n], func=AF.Square), reads=[("ZC", j)], writes=[("lsq", r)])
                    S.op("pe", lambda e, j=j, r=r, b2=b2, n=n: e.matmul(ps[b2][:, 0:n], lhsT=ones[:], rhs=lsq[:, r, 0:n], start=(j == 0), stop=(j == 3)),
                         reads=[("lsq", r), "ones"], writes=[("ps", b2)])
                S.op("dve", lambda e, b1=b1, n=n: e.tensor_single_scalar(out=lm[:, 0:n], in_=ps[b1][:, 0:n], scalar=1.0 / 512, op=ALU.mult), reads=[("ps", b1)], writes=["lm"])
                S.op("dve", lambda e, n=n: e.tensor_tensor(out=lmsq[:, 0:n], in0=lm[:, 0:n], in1=lm[:, 0:n], op=ALU.mult), reads=["lm"], writes=["lmsq"])
                S.op("dve", lambda e, b2=b2, n=n: e.scalar_tensor_tensor(out=lrs[:, 0:n], in0=ps[b2][:, 0:n], scalar=1.0 / 512, in1=lmsq[:, 0:n], op0=ALU.mult, op1=ALU.subtract),
                     reads=[("ps", b2), "lmsq"], writes=["lrs"])
                S.op("act", lambda e, n=n: e.activation(out=lrs[:, 0:n], in_=lrs[:, 0:n], func=AF.Sqrt, bias=1e-5, scale=1.0), reads=["lrs"], writes=["lrs"])
                S.op("dve", lambda e, n=n: e.reciprocal(out=lrs[:, 0:n], in_=lrs[:, 0:n]), reads=["lrs"], writes=["lrs"])
                for j in range(4):
                    r = ring("lcen", 2)
                    S.op("dve", lambda e, j=j, r=r, pos=pos, n=n: e.tensor_tensor(out=lcen[:, r, 0:n], in0=ZC[:, j, pos:pos + n], in1=lm[:, 0:n], op=ALU.subtract),
                         reads=[("ZC", j), "lm"], writes=[("lcen", r)])
                    S.op("dve", lambda e, r=r, n=n: e.tensor_tensor(out=lcen[:, r, 0:n], in0=lcen[:, r, 0:n], in1=lrs[:, 0:n], op=ALU.mult),
                         reads=[("lcen", r), "lrs"], writes=[("lcen", r)])
                    S.op("act", lambda e, j=j, r=r, col=col, n=n, l=l: e.activation(out=ybr[:, 12 + j, col:col + n], in_=lcen[:, r, 0:n], func=AF.Silu,
                                                                                 scale=V("clg", l, j), bias=V("clb", l, j)),
                         reads=[("lcen", r), "vecs"], writes=[("ybr", 3, j, col)])
            S.barrier()

            if stop_after == "conf":
                break
            sgr = view(A, AU, F32, [12, 512])
            accf = view(A, AU + 24576, F32, [2, 512])
            tmpm = view(A, AU + 28672, F32, [4, 512])
            for dc in range(16):
                for half in range(2):
                    sg = wload(("g", l, dc, half))
                    for gi_, (c0, n) in enumerate(gout):
                        for cc in range(2):
                            i = half * 2 + cc
                            b = nb()
                            proj(sg, cc, 2, c0, n, b)
                            r = i * 3 + gi_
                            S.op("act", lambda e, b=b, r=r, n=n: e.activation(out=sgr[:, r, 0:n], in_=ps[b][:, 0:n], func=AF.Sigmoid), reads=[("ps", b)], writes=[("sgr", r)])
                sb_ = wload(("wb", l, dc))
                for gi_, (c0, n) in enumerate(gout):
                    pbk = []
                    for i in range(4):
                        b = nb()

                        def mmp(e, i=i, b=b, c0=c0, n=n, sb_=sb_):
                            for kc in range(4):
                                o = (i * 4 + kc) * 128
                                ins = e.matmul(ps[b][:, 0:n], lhsT=wsl[sb_][:, o:o + 128], rhs=ybr[:, i * 4 + kc, c0:c0 + n], start=(kc == 0), stop=(kc == 3))
                            return ins
                        S.op("pe", mmp, reads=[wkey(sb_)], writes=[("ps", b)])
                        pbk.append(b)
                    sgk = [i * 3 + gi_ for i in range(4)]
                    ra = ring("accf", 2)
                    S.op("dve", lambda e, ra=ra, n=n, b=pbk[0], r=sgk[0]: e.tensor_tensor(out=accf[:, ra, 0:n], in0=ps[b][:, 0:n], in1=sgr[:, r, 0:n], op=ALU.mult),
                         reads=[("ps", pbk[0]), ("sgr", sgk[0])], writes=[("accf", ra)])
                    for i in range(1, 4):
                        rt = ring("tmpm", 4)
                        S.op("dve", lambda e, rt=rt, n=n, b=pbk[i], r=sgk[i]: e.tensor_tensor(out=tmpm[:, rt, 0:n], in0=ps[b][:, 0:n], in1=sgr[:, r, 0:n], op=ALU.mult),
                             reads=[("ps", pbk[i]), ("sgr", sgk[i])], writes=[("tmpm", rt)])
                        if i < 3:
                            S.op("dve", lambda e, rt=rt, ra=ra, n=n: e.tensor_tensor(out=accf[:, ra, 0:n], in0=accf[:, ra, 0:n], in1=tmpm[:, rt, 0:n], op=ALU.add),
                                 reads=[("accf", ra), ("tmpm", rt)], writes=[("accf", ra)])
                        else:
                            S.op("dve", lambda e, rt=rt, ra=ra, n=n, dc=dc, c0=c0: e.tensor_tensor(out=merged[:, dc, c0:c0 + n], in0=accf[:, ra, 0:n], in1=tmpm[:, rt, 0:n], op=ALU.add),
                                 reads=[("accf", ra), ("tmpm", rt)], writes=[("mg", dc, c0)])
            S.barrier()

            if stop_after == "mg":
                break
            stg = hT[:].rearrange("p k c -> p (k c)")[:, 0:3072].bitcast(F32).rearrange("p (a c) -> p a c", a=3)
            xspv = xsp.rearrange("p (k c) -> p k c", k=16)
            for pr in range(8):
                s = wload(("wo", l, pr))
                for cc in range(2):
                    d2 = pr * 2 + cc
                    for (c0, n) in gout:
                        r = ring("stg", 3)
                        S.op("sp", lambda e, r=r, d2=d2, c0=c0, n=n: e.dma_start(out=stg[:, r, 0:n], in_=xspv[:, d2, c0:c0 + n]),
                             reads=["xsp"], writes=[("stg", r)], dma="stg%d" % r)
                        b = nb()

                        def mmw(e, s=s, cc=cc, b=b, c0=c0, n=n):
                            for k in range(16):
                                o = (k * 2 + cc) * 128
                                ins = e.matmul(ps[b][:, 0:n], lhsT=wsl[s][:, o:o + 128], rhs=merged[:, k, c0:c0 + n], start=(k == 0), stop=(k == 15))
                            return ins
                        S.op("pe", mmw, reads=[wkey(s)] + [("mg", k, c0) for k in range(16)], writes=[("ps", b)])
                        for (s0, sn, kind) in segs_of(c0, n):
                            t = 0 if kind == "x" else 1
                            S.op("dve", lambda e, b=b, r=r, d2=d2, c0=c0, s0=s0, sn=sn, mc=modcol(t, 2, d2, l): e.scalar_tensor_tensor(
                                out=xT[:, d2, s0:s0 + sn], in0=ps[b][:, s0 - c0:s0 - c0 + sn], scalar=mc, in1=stg[:, r, s0 - c0:s0 - c0 + sn],
                                op0=ALU.mult, op1=ALU.add), reads=[("ps", b), ("stg", r), ("modp", 0), ("modp", 1)], writes=xkeys(d2, c0, n))
            S.barrier()
            if stop_after == "mix%d" % l:
                break

            lgT = view(C, 0, F32, [NRES])
            LG = view(C, 5632, F32, [11, 16])
            RT = view(C, 6400, F32, [20, 16])
            COMB = view(C, 7680, F32, [11, 16])
            cTs = view(C, 8384, F32, [NRES])

            def router(c0, n, rb):
                S.op("act", lambda e: e.activation(out=lgT[0:16, c0:c0 + n], in_=ps[rb][0:16, 0:n], func=AF.Copy), reads=[("ps", rb)], writes=[("lgT", c0)])
                b = nb()

                def trl(e):
                    for tt in range(n // 128):
                        ins = e.transpose(out=ps[b][:, tt * 16:(tt + 1) * 16], in_=lgT[0:16, c0 + tt * 128:c0 + (tt + 1) * 128], identity=ident[0:16, 0:16])
                    return ins
                S.op("pe", trl, reads=[("lgT", c0), "ident"], writes=[("ps", b)])
                t0 = c0 // 128
                nt = n // 128
                S.op("act", lambda e: e.activation(out=LG[:, t0:t0 + nt, :], in_=ps[b][:, 0:nt * 16].rearrange("p (t c) -> p t c", t=nt), func=AF.Copy),
                     reads=[("ps", b)], writes=[("LG", c0)])
            norm_phase(gout, lambda k, c0, n: xT[:, k, c0:c0 + n], xkeys, 1, 3, router=router)
            ntl = 11 if l == 0 else 8

            def routing_block(nt, gout):
                RB = view(C, 14016, F32, [9, 11, 16])
                RS = view(C, 14016 + 6336, F32, [9, 11, 4])
                lk = [("LG", c0) for (c0, n) in gout]
                T = LG[:, 0:nt, :]
                big = lambda i: RB[:, i, 0:nt, :]
                smv = lambda i, w: RS[:, i, 0:nt, 0:w]
                sm1 = lambda i: RS[:, i, 0:nt, 0]
                bc16 = lambda i: RS[:, i, 0:nt, 0:1].to_broadcast([128, nt, 16])
                g44 = lambda ap: ap.rearrange("p t (g c) -> p t g c", g=4)
                bk = lambda i: ("RB", i)
                sk = lambda i: ("RS", i)

                def dv(fn, rd, wr):
                    S.op("dve", fn, reads=rd, writes=wr)
                dv(lambda e: e.tensor_reduce(out=sm1(0), in_=T, axis=AX.X, op=ALU.max), lk, [sk(0)])
                dv(lambda e: e.tensor_tensor(out=big(0), in0=T, in1=bc16(0), op=ALU.subtract), lk + [sk(0)], [bk(0)])
                S.op("act", lambda e: e.activation(out=big(0), in_=big(0), func=AF.Exp), reads=[bk(0)], writes=[bk(0)])
                dv(lambda e: e.tensor_reduce(out=sm1(1), in_=big(0), axis=AX.X, op=ALU.add), [bk(0)], [sk(1)])
                dv(lambda e: e.reciprocal(out=sm1(1), in_=sm1(1)), [sk(1)], [sk(1)])
                dv(lambda e: e.tensor_tensor(out=big(1), in0=big(0), in1=bc16(1), op=ALU.mult), [bk(0), sk(1)], [bk(1)])
                dv(lambda e: e.tensor_tensor(out=big(2), in0=big(1), in1=rbb[:].unsqueeze(1).to_broadcast([128, nt, 16]), op=ALU.add), [bk(1), "rbb"], [bk(2)])
                dv(lambda e: e.tensor_reduce(out=smv(2, 4), in_=g44(big(2)), axis=AX.X, op=ALU.max), [bk(2)], [sk(2)])
                dv(lambda e: e.tensor_tensor(out=g44(big(3)), in0=g44(big(2)), in1=smv(2, 4).unsqueeze(3).to_broadcast([128, nt, 4, 4]), op=ALU.is_equal), [bk(2), sk(2)], [bk(3)])
                dv(lambda e: e.scalar_tensor_tensor(out=big(3), in0=big(3), scalar=NEG, in1=big(2), op0=ALU.mult, op1=ALU.add), [bk(3), bk(2)], [bk(3)])
                dv(lambda e: e.tensor_reduce(out=smv(3, 4), in_=g44(big(3)), axis=AX.X, op=ALU.max), [bk(3)], [sk(3)])
                dv(lambda e: e.tensor_tensor(out=smv(3, 4), in0=smv(3, 4), in1=smv(2, 4), op=ALU.add), [sk(3), sk(2)], [sk(3)])
                dv(lambda e: e.tensor_reduce(out=sm1(4), in_=smv(3, 4), axis=AX.X, op=ALU.max), [sk(3)], [sk(4)])
                dv(lambda e: e.tensor_tensor(out=smv(5, 4), in0=smv(3, 4), in1=RS[:, 4, 0:nt, 0:1].to_broadcast([128, nt, 4]), op=ALU.is_equal), [sk(3), sk(4)], [sk(5)])
                dv(lambda e: e.tensor_scalar(out=smv(5, 4), in0=smv(5, 4), scalar1=1.0, scalar2=-NEG, op0=ALU.subtract, op1=ALU.mult), [sk(5)], [sk(5)])
                dv(lambda e: e.tensor_tensor(out=g44(big(4)), in0=g44(big(2)), in1=smv(5, 4).unsqueeze(3).to_broadcast([128, nt, 4, 4]), op=ALU.add), [bk(2), sk(5)], [bk(4)])
                dv(lambda e: e.tensor_reduce(out=sm1(6), in_=big(4), axis=AX.X, op=ALU.max), [bk(4)], [sk(6)])
                dv(lambda e: e.tensor_tensor(out=big(5), in0=big(4), in1=bc16(6), op=ALU.is_equal), [bk(4), sk(6)], [bk(5)])
                dv(lambda e: e.scalar_tensor_tensor(out=big(6), in0=big(5), scalar=NEG, in1=big(4), op0=ALU.mult, op1=ALU.add), [bk(5), bk(4)], [bk(6)])
                dv(lambda e: e.tensor_reduce(out=sm1(7), in_=big(6), axis=AX.X, op=ALU.max), [bk(6)], [sk(7)])
                dv(lambda e: e.tensor_tensor(out=big(7), in0=big(6), in1=bc16(7), op=ALU.is_equal), [bk(6), sk(7)], [bk(7)])
                dv(lambda e: e.tensor_tensor(out=big(7), in0=big(7), in1=big(5), op=ALU.add), [bk(7), bk(5)], [bk(7)])
                dv(lambda e: e.tensor_tensor(out=big(8), in0=big(7), in1=big(1), op=ALU.mult), [bk(7), bk(1)], [bk(8)])
                dv(lambda e: e.tensor_reduce(out=sm1(8), in_=big(8), axis=AX.X, op=ALU.add), [bk(8)], [sk(8)])
                dv(lambda e: e.reciprocal(out=sm1(8), in_=sm1(8)), [sk(8)], [sk(8)])
                dv(lambda e: e.tensor_tensor(out=COMB[:, 0:nt, :], in0=big(8), in1=bc16(8), op=ALU.mult), [bk(8), sk(8)], [("COMB", ti) for ti in range(nt)])

            routing_block(ntl, gout)
            for (c0, n) in gout:
                b = nb()
                t0 = c0 // 128

                def trc(e, b=b, t0=t0, n=n):
                    for tt in range(n // 128):
                        ins = e.transpose(out=ps[b][0:16, tt * 128:(tt + 1) * 128], in_=COMB[:, t0 + tt, :], identity=ident[:])
                    return ins
                S.op("pe", trc, reads=[("COMB", t0 + tt) for tt in range(n // 128)] + ["ident"], writes=[("ps", b)])
                S.op("act", lambda e, b=b, c0=c0, n=n: e.activation(out=cTs[0:16, c0:c0 + n], in_=ps[b][0:16, 0:n], func=AF.Copy), reads=[("ps", b)], writes=[("cTs", c0)])
            ntok = gout[-1][0] + gout[-1][1]
            S.op("sp", lambda e, ntok=ntok: e.dma_start(out=ctd[:, 0:ntok], in_=cTs[0:16, 0:ntok]), reads=[("cTs", c0) for (c0, n) in gout], writes=["ctd"], dma="ctd")
            S.barrier()

            actT = view(C, 0, BF16, [8, NRES])
            cbc = view(C, 22528, F32, [2, NRES])
            sat = view(C, 33792, F32, [2, 512])
            tbt = view(C, 37888, F32, [2, 512])
            for ex in range(16):
                cb = ex % 2
                S.op("sp", lambda e, ex=ex, cb=cb, ntok=ntok: e.dma_start(out=cbc[:, cb, 0:ntok], in_=ctd[ex, 0:ntok].partition_broadcast(128)),
                     reads=["ctd"], writes=[("cbc", cb)], dma="cbc%d" % cb)
                for fc in range(8):
                    s = wload(("up", l, ex, fc))
                    for (c0, n) in gout:
                        ba, bb = nb(), nb()
                        proj(s, 0, 2, c0, n, ba)
                        proj(s, 1, 2, c0, n, bb)
                        r = ring("sat", 2)
                        S.op("act", lambda e, ba=ba, r=r, n=n: e.activation(out=sat[:, r, 0:n], in_=ps[ba][:, 0:n], func=AF.Silu), reads=[("ps", ba)], writes=[("sat", r)])
                        S.op("dve", lambda e, bb=bb, r=r, n=n: e.tensor_tensor(out=tbt[:, r, 0:n], in0=ps[bb][:, 0:n], in1=sat[:, r, 0:n], op=ALU.mult),
                             reads=[("ps", bb), ("sat", r)], writes=[("tbt", r)])
                        S.op("dve", lambda e, r=r, fc=fc, cb=cb, c0=c0, n=n: e.tensor_tensor(out=actT[:, fc, c0:c0 + n], in0=tbt[:, r, 0:n], in1=cbc[:, cb, c0:c0 + n], op=ALU.mult),
                             reads=[("tbt", r), ("cbc", cb)], writes=[("actT", fc, c0)])
                for db in range(4):
                    s = wload(("dn", l, ex, db))
                    for cc in range(4):
                        d2 = db * 4 + cc
                        for (c0, n) in gout:
                            b = nb()

                            def mmdn(e, s=s, cc=cc, b=b, c0=c0, n=n):
                                for fk in range(8):
                                    o = fk * 512 + cc * 128
                                    ins = e.matmul(ps[b][:, 0:n], lhsT=wsl[s][:, o:o + 128], rhs=actT[:, fk, c0:c0 + n], start=(fk == 0), stop=(fk == 7))
                                return ins
                            S.op("pe", mmdn, reads=[wkey(s)] + [("actT", fk, c0) for fk in range(8)], writes=[("ps", b)])
                            for (s0, sn, kind) in segs_of(c0, n):
                                t = 0 if kind == "x" else 1
                                S.op("dve", lambda e, b=b, d2=d2, c0=c0, s0=s0, sn=sn, mc=modcol(t, 5, d2, l): e.scalar_tensor_tensor(
                                    out=xT[:, d2, s0:s0 + sn], in0=ps[b][:, s0 - c0:s0 - c0 + sn], scalar=mc, in1=xT[:, d2, s0:s0 + sn],
                                    op0=ALU.mult, op1=ALU.add), reads=[("ps", b), ("modp", 0), ("modp", 1)] + xkeys(d2, c0, n), writes=xkeys(d2, c0, n))
            S.barrier()
            if stop_after == "moe%d" % l:
                break

        allx = [("xT", k, g) for k in range(16) for g in range(3)]
        if stop_after is not None:
            S.op("sp", lambda e: e.dma_start(out=dbg_d, in_=A[:, 0:16 * NRES * 4].bitcast(F32)), reads=allx, writes=["dbg"], dma="dbg")
            S.op("sp", None, reads=["dbg"])
        else:
            fgr = [(0, 512), (512, 512)]
            norm_phase(fgr, lambda k, c0, n: xT[:, k, c0:c0 + n], xkeys, 0, 0, final=True)
            for (c0, n) in fgr:
                for k in range(16):
                    S.op("dve", lambda e, k=k, c0=c0, n=n: e.scalar_tensor_tensor(out=xT[:, k, c0:c0 + n], in0=xT[:, k, c0:c0 + n], scalar=V("fg", 0, k),
                                                                                 in1=rstd[:, c0:c0 + n], op0=ALU.mult, op1=ALU.mult),
                         reads=xkeys(k, c0, n) + [("rstd", c0), "vecs"], writes=xkeys(k, c0, n))
            ostg = view(C, 0, F32, [2, D])
            for t in range(8):
                ob = t % 2
                for k4 in range(4):
                    b = nb()

                    def tro(e, t=t, k4=k4, b=b):
                        for kk in range(4):
                            ins = e.transpose(out=ps[b][:, kk * 128:(kk + 1) * 128], in_=xT[:, k4 * 4 + kk, t * 128:(t + 1) * 128], identity=ident[:])
                        return ins
                    S.op("pe", tro, reads=[("xT", k4 * 4 + kk, t // 4) for kk in range(4)] + ["ident"], writes=[("ps", b)])
                    S.op("act" if k4 % 2 == 0 else "dve",
                         (lambda e, ob=ob, k4=k4, b=b: e.activation(out=ostg[:, ob, k4 * 512:(k4 + 1) * 512], in_=ps[b][:], func=AF.Copy))
                         if k4 % 2 == 0 else
                         (lambda e, ob=ob, k4=k4, b=b: e.tensor_copy(out=ostg[:, ob, k4 * 512:(k4 + 1) * 512], in_=ps[b][:])),
                         reads=[("ps", b)], writes=[("ostg", ob, k4)])
                S.op("sp", lambda e, t=t, ob=ob: e.dma_start(out=out_d[t * 128:(t + 1) * 128, :], in_=ostg[:, ob, :]),
                     reads=[("ostg", ob, k4) for k4 in range(4)], writes=[("out", t)], dma="out%d" % ob)
            S.op("sp", None, reads=[("out", t) for t in range(8)])
        S.emit(st)
        nc._sched_stats = (len(S.ops), S.ecount)
        nc._plan = plan
    return nc


def host_inputs(inp, plan, ncores=8):
    x = np.asarray(inp["x"], np.float32)
    ctx = np.asarray(inp["ctx"], np.float32)
    c = np.asarray(inp["c"], np.float32)
    c_ctx = np.asarray(inp["c_ctx"], np.float32)
    wts = pack_weights({k: np.asarray(v) for k, v in inp.items()}, plan)
    half = 32
    inv = (1.0 / (10000.0 ** (np.arange(0, half, 2, dtype=np.float32) / half))).astype(np.float32)
    colf = lambda v: np.ascontiguousarray(np.asarray(v, np.float32).reshape(-1, 128).T)
    maps = []
    for core in range(ncores):
        b, typ = core // 2, core % 2
        pos = np.arange(1280) if typ == 0 else 2047 - np.arange(1280)
        xs = x[b][pos]
        cx = ctx[b] if typ == 0 else ctx[b][::-1]
        xin = np.concatenate([xs[:1152], cx, xs[1152:1280]], axis=0)
        posc = np.concatenate([pos[:1152], np.zeros(256, np.int64), pos[1152:1280]])
        row = (posc // 64).astype(np.float32)
        colp = (posc % 64).astype(np.float32)
        Ct = np.ones((128, NCOL), np.float32)
        St = np.zeros((128, NCOL), np.float32)
        for p in range(128):
            d = p % 64
            f = (d % 32) % 16
            ang = (row if d < 32 else colp) * inv[f]
            Ct[p] = np.cos(ang)
            sn = np.sin(ang)
            St[p] = -sn if (d % 32) < 16 else sn
        Ct[:, 1152:1408] = 1.0
        St[:, 1152:1408] = 0.0
        rope = np.concatenate([Ct, St], axis=1)
        jj = np.arange(128)[:, None]
        ii = np.arange(128)[None, :]
        mp = (ii <= jj).astype(np.float32)
        mn = (jj <= ii).astype(np.float32)
        masks = np.concatenate([np.tile(mp, (1, 4)), np.tile(mn, (1, 4))], axis=1)
        cin = np.stack([colf(c[b]), colf(c_ctx)], axis=2).reshape(128, 32)
        vecs = np.zeros((128, NVEC), np.float32)
        rowv = np.zeros((2, 1536), np.float32)
        sguw = np.zeros((2 * 128, 512), np.float32)
        for l in range(2):
            vecs[:, VOFF[("adab", l)]:VOFF[("adab", l)] + 96] = colf(inp["ada_b"][l])
            vecs[:, VOFF[("n1g", l)]:VOFF[("n1g", l)] + 16] = colf(inp["norm1_g"][l])
            vecs[:, VOFF[("n2g", l)]:VOFF[("n2g", l)] + 16] = colf(inp["norm2_g"][l])
            sk = np.asarray(inp["attn_sink"][l], np.float32)
            for j in range(4):
                vecs[0:64, VOFF[("sink", l)] + j] = sk[2 * j]
                vecs[64:128, VOFF[("sink", l)] + j] = sk[2 * j + 1]
            cdw = np.asarray(inp["conf_dw_w"][l], np.float32)
            scw = np.asarray(inp["sconv_w"][l], np.float32)
            if typ == 1:
                cdw = cdw[::-1]
                scw = scw[::-1]
            for j in range(4):
                vecs[:, VOFF[("cdw", l)] + j * 31:VOFF[("cdw", l)] + (j + 1) * 31] = cdw[:, j * 128:(j + 1) * 128].T
                vecs[:, VOFF[("scw", l)] + j * 3:VOFF[("scw", l)] + (j + 1) * 3] = scw[:, j * 128:(j + 1) * 128].T
            vecs[:, VOFF[("cdb", l)]:VOFF[("cdb", l)] + 4] = colf(inp["conf_dw_b"][l])
            vecs[:, VOFF[("clg", l)]:VOFF[("clg", l)] + 4] = colf(inp["conf_ln_g"][l])
            vecs[:, VOFF[("clb", l)]:VOFF[("clb", l)] + 4] = colf(inp["conf_ln_b"][l])
            rowv[l, 0:512] = inp["sgu_ln_g"][l]
            rowv[l, 512:1024] = inp["sgu_ln_b"][l]
            ws = np.asarray(inp["sgu_w"][l], np.float32)
            bs = np.asarray(inp["sgu_b"][l], np.float32)
            if typ == 1:
                ws = ws[:, ::-1, ::-1]
                bs = bs[:, ::-1]
            rowv[l, 1024:1536] = bs.reshape(-1)
            sguw[l * 128:(l + 1) * 128] = ws.transpose(2, 0, 1).reshape(128, 512)
        vecs[:, VOFF[("fg", 0)]:VOFF[("fg", 0)] + 16] = colf(inp["final_g"])
        rwl = np.asarray(inp["router_w"], np.float32).reshape(16, 128, 16).transpose(1, 0, 2).reshape(128, 256)
        maps.append(dict(xin=np.ascontiguousarray(xin), wts=wts, rope=rope, masks=masks, cin=np.ascontiguousarray(cin),
                         vecs=vecs, rowv=rowv, sguw=sguw, rw=np.ascontiguousarray(rwl),
                         rb=np.asarray(inp["router_b"], np.float32).reshape(1, 16)))
    return maps


def kernel(**inputs):
    nc = build()
    assert len(nc._plan) == NBLK
    maps = host_inputs(inputs, nc._plan)
    res = run_bass_kernel_spmd(nc, maps, core_ids=list(range(8)))
    out = np.zeros((4, 2048, D), np.float32)
    for core in range(8):
        b, typ = core // 2, core % 2
        o = res.results[core]["out"]
        if typ == 0:
            out[b, 0:1024] = o
        else:
            out[b, 1024:2048] = o[::-1]
    return out
```

```python
import math
from contextlib import ExitStack
import numpy as np
import concourse.bass as bass
import concourse.mybir as mybir
from concourse.bass_utils import run_bass_kernel_spmd

F32 = mybir.dt.float32
BF16 = mybir.dt.bfloat16
U8 = mybir.dt.uint8
AF = mybir.ActivationFunctionType
ALU = mybir.AluOpType
AX = mybir.AxisListType

D = 2048
NCOL = 1536
NRES = 1408
NW = 2
WSZ = 4096
NEG = -1.0e30


class Sched:
    ENGS = ("pe", "act", "dve", "pool", "sp")

    def __init__(self, nc):
        self.nc = nc
        self.ops = []
        self.last_w = {}
        self.readers = {}
        self.bar = None
        self.last_on = {}
        self.dmas_since = []

    def op(self, eng, fn, reads=(), writes=(), dma=None, nobar=False):
        i = len(self.ops)
        deps = set()
        psr = [k for k in reads if isinstance(k, tuple) and k[0] == "ps"]
        if psr:
            reads = [k for k in reads if not (isinstance(k, tuple) and k[0] == "ps")]
            writes = list(writes) + psr
        for k in reads:
            j = self.last_w.get(k)
            if j is not None:
                deps.add(j)
        for k in writes:
            j = self.last_w.get(k)
            if j is not None:
                deps.add(j)
            for r in self.readers.get(k, ()):
                deps.add(r)
        if self.bar is not None and not nobar:
            deps.add(self.bar)
        deps.discard(i)
        self.ops.append(dict(eng=eng, fn=fn, deps=deps, dma=dma, needed=False, seq=None, dval=None))
        for k in reads:
            self.readers.setdefault(k, []).append(i)
        for k in writes:
            self.last_w[k] = i
            self.readers[k] = []
        if not nobar:
            if dma is not None:
                self.dmas_since.append(i)
            else:
                self.last_on[eng] = i
        return i

    def barrier(self):
        i = len(self.ops)
        deps = set(self.last_on.values()) | set(self.dmas_since)
        if self.bar is not None:
            deps.add(self.bar)
        self.ops.append(dict(eng="act", fn=self.barfn, deps=deps, dma=None, needed=False, seq=None, dval=None))
        self.bar = i
        self.last_on = {}
        self.dmas_since = []
        return i

    def emit(self, stack):
        nc = self.nc
        ops = self.ops
        for o in ops:
            for j in o["deps"]:
                pj = ops[j]
                if pj["dma"] is None and pj["eng"] == "pe" and o["eng"] == "pe" and o["dma"] is None and pj["fn"] is not None:
                    continue
                pj["needed"] = True
        esem = {e: stack.enter_context(nc.semaphore("s_" + e)) for e in self.ENGS}
        dsem = {}
        ecount = {e: 0 for e in self.ENGS}
        dcount = {}
        for o in ops:
            if o["dma"] is not None:
                if o["dma"] not in dsem:
                    dsem[o["dma"]] = stack.enter_context(nc.semaphore("d_%d" % len(dsem)))
                    dcount[o["dma"]] = 0
                dcount[o["dma"]] += 16
                o["dval"] = dcount[o["dma"]]
            elif o["needed"]:
                ecount[o["eng"]] += 1
                o["seq"] = ecount[o["eng"]]
        streams = {e: [] for e in self.ENGS}
        waited = {e: {} for e in self.ENGS}
        for i, o in enumerate(ops):
            e = o["eng"]
            ws = []
            for j in sorted(o["deps"]):
                pj = ops[j]
                if pj["dma"] is not None:
                    sem, val, key = dsem[pj["dma"]], pj["dval"], ("d", pj["dma"])
                else:
                    if pj["eng"] == "pe" and e == "pe" and o["dma"] is None and pj["fn"] is not None:
                        continue
                    sem, val, key = esem[pj["eng"]], pj["seq"], ("e", pj["eng"])
                if waited[e].get(key, 0) >= val:
                    continue
                waited[e][key] = val
                ws.append((sem, val))
            streams[e].append((ws, o))
        self.ecount = ecount

        def run(eng, items):
            for ws, o in items:
                for sem, val in ws:
                    eng.wait_ge(sem, val)
                if o["fn"] is None:
                    if o["needed"]:
                        eng.sem_inc(esem[o["eng"]], 1)
                    continue
                r = o["fn"](eng)
                if o["dma"] is not None:
                    r.then_inc(dsem[o["dma"]], 16)
                elif o["needed"]:
                    r.then_inc(esem[o["eng"]], 1)

        with nc.Block() as block:
            @block.sync
            def _(eng):
                run(eng, streams["sp"])

            @block.tensor
            def _(eng):
                run(eng, streams["pe"])

            @block.scalar
            def _(eng):
                run(eng, streams["act"])

            @block.vector
            def _(eng):
                run(eng, streams["dve"])

            @block.gpsimd
            def _(eng):
                run(eng, streams["pool"])


SEC = dict(q=0, k=512, v=640, su=768, sv=1280, cb=1792, cc=2304, cx=2816, ga=3328, gb=3840, gate=4352)


def rope_partner():
    p = np.zeros(64, np.int64)
    for d in range(64):
        p[d] = d + 16 if (d % 32) < 16 else d - 16
    return p


def in_chunks():
    part = rope_partner()
    ch = {}
    for j in range(4):
        base = SEC["q"] + j * 128
        ch["Q%d" % j] = base + np.arange(128)
        ch["Qs%d" % j] = base + np.concatenate([part, 64 + part])
    for g, nm in enumerate("AB"):
        base = SEC["k"] + g * 64
        ch["K" + nm] = base + np.concatenate([np.arange(64), np.arange(64)])
        ch["Ks" + nm] = base + np.concatenate([part, part])
    for j in range(4):
        ch["u%d" % j] = SEC["su"] + j * 128 + np.arange(128)
        ch["B%d" % j] = SEC["cb"] + j * 128 + np.arange(128)
        ch["C%d" % j] = SEC["cc"] + j * 128 + np.arange(128)
        ch["x%d" % j] = SEC["cx"] + j * 128 + np.arange(128)
        ch["a%d" % j] = SEC["ga"] + j * 128 + np.arange(128)
        ch["b%d" % j] = SEC["gb"] + j * 128 + np.arange(128)
    return ch


IN_BLOCKS = [("Q0", "Qs0"), ("Q1", "Qs1"), ("Q2", "Qs2"), ("Q3", "Qs3"), ("KA", "KsA"), ("KB", "KsB"),
             ("V",), ("u0", "u1"), ("u2", "u3"), ("SV0",), ("SV1",),
             ("B0", "C0"), ("x0", "B1"), ("C1", "x1"), ("B2", "C2"), ("x2", "B3"), ("C3", "x3"),
             ("a0", "b0"), ("a1", "b1"), ("a2", "b2"), ("a3", "b3")]


def pack_weights(inp, plan):
    nblk = len(plan)
    out = np.zeros((nblk, 128, WSZ), np.float32)
    ch = in_chunks()
    for bi, d in enumerate(plan):
        kind, l = d[0], d[1]
        if kind == "ada":
            aw = inp["ada_w"][l].reshape(16, 128, 6 * D)
            b = d[2]
            out[bi] = aw[:, :, b * 256:(b + 1) * 256].transpose(1, 0, 2).reshape(128, WSZ)
        elif kind == "in":
            win = inp["w_in"][l].reshape(16, 128, -1)
            blk = IN_BLOCKS[d[2]]
            if blk[0] == "V":
                cols = SEC["v"] + np.arange(128)
                out[bi, :, :2048] = win[:, :, cols].transpose(1, 0, 2).reshape(128, 2048)
            elif blk[0].startswith("SV"):
                h = int(blk[0][2])
                cols = SEC["sv"] + h * 256 + np.arange(256)
                out[bi] = win[:, :, cols].transpose(1, 0, 2).reshape(128, WSZ)
            else:
                cols = np.concatenate([ch[blk[0]], ch[blk[1]]])
                out[bi] = win[:, :, cols].transpose(1, 0, 2).reshape(128, WSZ)
        elif kind == "g":
            win = inp["w_in"][l].reshape(16, 128, -1)
            dc, half = d[2], d[3]
            cols = np.concatenate([SEC["gate"] + gi * D + dc * 128 + np.arange(128) for gi in (2 * half, 2 * half + 1)])
            out[bi] = win[:, :, cols].transpose(1, 0, 2).reshape(128, WSZ)
        elif kind == "wb":
            wbr = inp["w_branch"][l].reshape(4, 4, 128, D)
            dc = d[2]
            out[bi, :, :2048] = wbr[:, :, :, dc * 128:(dc + 1) * 128].transpose(2, 0, 1, 3).reshape(128, 2048)
        elif kind == "wo":
            wo = inp["w_out"][l].reshape(16, 128, D)
            pr = d[2]
            out[bi] = wo[:, :, pr * 256:(pr + 1) * 256].transpose(1, 0, 2).reshape(128, WSZ)
        elif kind == "up":
            e, fc = d[2], d[3]
            wu = inp["exp_w_up"][l][e].reshape(16, 128, 2, 1024)
            out[bi] = wu[:, :, :, fc * 128:(fc + 1) * 128].transpose(1, 0, 2, 3).reshape(128, WSZ)
        elif kind == "dn":
            e, db = d[2], d[3]
            wd = inp["exp_w_down"][l][e].reshape(8, 128, D)
            out[bi] = wd[:, :, db * 512:(db + 1) * 512].transpose(1, 0, 2).reshape(128, WSZ)
    return out.reshape(nblk * 128, WSZ)


def vec_layout():
    off = {}
    o = 0
    for l in range(2):
        for nm, n in (("adab", 96), ("n1g", 16), ("n2g", 16), ("sink", 4), ("cdw", 124), ("cdb", 4),
                      ("clg", 4), ("clb", 4), ("scw", 12)):
            off[(nm, l)] = o
            o += n
    off[("fg", 0)] = o
    o += 16
    return off, o


VOFF, NVEC = vec_layout()

TCOL = [128 * t for t in range(9)] + [1408]
CTXC = [1152, 1280]
SEGS = [(0, 1152, "x"), (1152, 1408, "c"), (1408, 1536, "x")]
CPOS = [(0, 1152, 16), (1152, 1408, 1312), (1408, 1536, 1168)]
CONVL = 1584


def segs_of(c0, n):
    r = []
    for a, b, kind in SEGS:
        lo, hi = max(a, c0), min(b, c0 + n)
        if lo < hi:
            r.append((lo, hi - lo, kind))
    return r


def pieces_of(c0, n):
    r = []
    for a, b, pos in CPOS:
        lo, hi = max(a, c0), min(b, c0 + n)
        if lo < hi:
            r.append((lo, hi - lo, pos + lo - a))
    return r


NBLK = 634


def build(stop_after=None, nblk=NBLK):
    nc = bass.Bass("TRN2", target_bir_lowering=False)
    xin = nc.dram_tensor("xin", [NCOL, D], F32, kind="ExternalInput").ap()
    wts = nc.dram_tensor("wts", [nblk * 128, WSZ], F32, kind="ExternalInput").ap()
    rope_d = nc.dram_tensor("rope", [128, 2 * NCOL], F32, kind="ExternalInput").ap()
    mask_d = nc.dram_tensor("masks", [128, 1024], F32, kind="ExternalInput").ap()
    cin_d = nc.dram_tensor("cin", [128, 32], F32, kind="ExternalInput").ap()
    vecs_d = nc.dram_tensor("vecs", [128, NVEC], F32, kind="ExternalInput").ap()
    rowv_d = nc.dram_tensor("rowv", [2, 1536], F32, kind="ExternalInput").ap()
    sguw_d = nc.dram_tensor("sguw", [2 * 128, 512], F32, kind="ExternalInput").ap()
    rw_d = nc.dram_tensor("rw", [128, 256], F32, kind="ExternalInput").ap()
    rb_d = nc.dram_tensor("rb", [1, 16], F32, kind="ExternalInput").ap()
    out_d = nc.dram_tensor("out", [1024, D], F32, kind="ExternalOutput").ap()
    dbg_d = nc.dram_tensor("dbg", [128, 16 * NRES], F32, kind="ExternalOutput").ap() if stop_after is not None else None
    xsp = nc.dram_tensor("xsp", [128, 16 * NRES], F32, kind="Internal").ap()
    ctd = nc.dram_tensor("ctd", [16, NRES], F32, kind="Internal").ap()

    st = ExitStack()
    with st:
        S = Sched(nc)
        S.barfn = lambda e: e.activation(out=sml[:, 0:1], in_=sml[:, 1:2], func=AF.Copy)
        A = st.enter_context(nc.sbuf_tensor("A", [128, 90112], U8))
        C = st.enter_context(nc.sbuf_tensor("C", [128, 45056], U8))
        hT = st.enter_context(nc.sbuf_tensor("hT", [128, 16, NCOL], BF16))
        wsl = [st.enter_context(nc.sbuf_tensor("w%d" % i, [128, WSZ], BF16)) for i in range(NW)]
        vecs = st.enter_context(nc.sbuf_tensor("vecs_sb", [128, NVEC], F32))
        modp = st.enter_context(nc.sbuf_tensor("modp", [128, 4, 96], F32))
        gsv = st.enter_context(nc.sbuf_tensor("gsv", [128, 8, 16], F32))
        ident = st.enter_context(nc.sbuf_tensor("ident", [128, 128], F32))
        ones = st.enter_context(nc.sbuf_tensor("ones", [128, 128], F32))
        oneslh = st.enter_context(nc.sbuf_tensor("oneslh", [128, 2, 128], BF16))
        identb = st.enter_context(nc.sbuf_tensor("identb", [128, 128], BF16))
        cbf = st.enter_context(nc.sbuf_tensor("cbf", [128, 16, 2], BF16))
        cf32 = st.enter_context(nc.sbuf_tensor("cf32", [128, 32], F32))
        rw = st.enter_context(nc.sbuf_tensor("rw_sb", [128, 16, 16], F32))
        rbb = st.enter_context(nc.sbuf_tensor("rbb", [128, 16], F32))
        esk = st.enter_context(nc.sbuf_tensor("esk", [128, 4], F32))
        sml = st.enter_context(nc.sbuf_tensor("sml", [128, 32], F32))
        ps = [st.enter_context(nc.psum_tensor("ps%d" % i, [128, 512], F32)) for i in range(8)]

        def view(ar, off, dt, shape):
            nel = int(np.prod(shape))
            sz = 4 if dt == F32 else 2
            v = ar[:, off:off + nel * sz].bitcast(dt)
            if len(shape) == 2:
                v = v.rearrange("p (a b) -> p a b", a=shape[0])
            elif len(shape) == 3:
                v = v.rearrange("p (a b c) -> p a b c", a=shape[0], b=shape[1])
            elif len(shape) == 4:
                v = v.rearrange("p (a b c d) -> p a b c d", a=shape[0], b=shape[1], c=shape[2])
            return v

        xT = view(A, 0, F32, [16, NRES])
        ybr = view(A, 0, BF16, [16, NRES])
        AU = 45056
        merged = view(C, 0, BF16, [16, NRES])

        bank_rr = [0]

        def nb():
            b = bank_rr[0]
            bank_rr[0] = (b + 1) % 7
            return b

        rings = {}

        def ring(name, n):
            i = rings.get(name, 0)
            rings[name] = i + 1
            return i % n

        wstate = dict(next=0)

        plan = []

        def wload(desc):
            b = wstate["next"]
            plan.append(desc)
            wstate["next"] = b + 1
            s = b % NW
            S.op("pool", lambda e, b=b, s=s: e.dma_start(out=wsl[s][:], in_=wts[b * 128:(b + 1) * 128, :]),
                 writes=[("w", s)], dma="w%d" % s, nobar=True)
            return s

        def wkey(s):
            return ("w", s)

        S.op("sp", lambda e: e.dma_start(out=vecs[:], in_=vecs_d), writes=["vecs"], dma="k1")
        S.op("sp", lambda e: e.dma_start(out=cf32[:], in_=cin_d), writes=["cf32"], dma="k2")
        S.op("sp", lambda e: e.dma_start(out=rw[:].rearrange("p k e -> p (k e)"), in_=rw_d), writes=["rw"], dma="k3")
        S.op("sp", lambda e: e.dma_start(out=rbb[:], in_=rb_d[0, :].partition_broadcast(128)), writes=["rbb"], dma="k4")
        S.op("dve", lambda e: e.memset(sml[:], 0.0), writes=["sml"])
        S.op("dve", lambda e: e.memset(ones[:], 1.0), writes=["ones"])
        S.op("dve", lambda e: e.memset(ident[:], 0.0), writes=["ident"])
        S.op("pool", lambda e: e.affine_select(out=ident[:], in_=ones[:], pattern=[[-1, 128]], compare_op=ALU.is_equal,
                                               fill=0.0, base=0, channel_multiplier=1), reads=["ones"], writes=["ident"])
        S.op("dve", lambda e: e.tensor_copy(out=identb[:], in_=ident[:]), reads=["ident"], writes=["identb"])
        S.op("dve", lambda e: e.memset(oneslh[:], 0.0), writes=["oneslh"])
        S.op("dve", lambda e: e.memset(oneslh[:, 0, 0:64], 1.0), writes=["oneslh"])
        S.op("dve", lambda e: e.memset(oneslh[:, 1, 64:128], 1.0), writes=["oneslh"])
        S.op("act", lambda e: e.activation(out=cbf[:].rearrange("p k t -> p (k t)"), in_=cf32[:], func=AF.Silu),
             reads=["cf32"], writes=["cbf"])

        def V(nm, l, j, n=1):
            o = VOFF[(nm, l)] + j
            return vecs[:, o:o + n]

        stage = view(C, 0, F32, [2, D])
        X9 = view(C, 16384, F32, [16, 128])
        for i in range(12):
            sb = i % 2
            S.op("sp", lambda e, i=i, sb=sb: e.dma_start(out=stage[:, sb, :], in_=xin[i * 128:(i + 1) * 128, :]),
                 writes=[("stage", sb)], dma="st%d" % sb)
            for k4 in range(4):
                b = nb()

                def tr(e, sb=sb, k4=k4, b=b):
                    for kk in range(4):
                        k = k4 * 4 + kk
                        ins = e.transpose(out=ps[b][:, kk * 128:(kk + 1) * 128], in_=stage[:, sb, k * 128:(k + 1) * 128],
                                          identity=ident[:])
                    return ins
                S.op("pe", tr, reads=[("stage", sb), "ident"], writes=[("ps", b)])
                if i < 11:
                    dst = xT[:, k4 * 4:(k4 + 1) * 4, i * 128:(i + 1) * 128]
                    wk = [("xT", k4 * 4 + kk, i // 4 if i < 8 else 2) for kk in range(4)]
                else:
                    dst = X9[:, k4 * 4:(k4 + 1) * 4, :]
                    wk = ["X9"]
                S.op("act" if k4 % 2 == 0 else "dve",
                     (lambda e, dst=dst, b=b: e.activation(out=dst, in_=ps[b][:].rearrange("p (a c) -> p a c", a=4), func=AF.Copy))
                     if k4 % 2 == 0 else
                     (lambda e, dst=dst, b=b: e.tensor_copy(out=dst, in_=ps[b][:].rearrange("p (a c) -> p a c", a=4))),
                     reads=[("ps", b)], writes=wk)

        def xkeys(k, c0, n):
            return [("xT", k, g) for g in range(3) if c0 < min(512 * (g + 1), NRES) and c0 + n > 512 * g]

        GOUT = {0: [(0, 512), (512, 512), (1024, 384)], 1: [(0, 512), (512, 512)]}
        GIN = {0: [(0, 512), (512, 512), (1024, 512)], 1: [(0, 512), (512, 512), (1024, 384)]}

        def ada_block(l, b):
            s = wload(("ada", l, b))
            for cc in range(2):
                chunk = b * 2 + cc

                def mm(e, s=s, cc=cc, chunk=chunk):
                    for k in range(16):
                        ins = e.matmul(ps[7][:, chunk * 2:chunk * 2 + 2], lhsT=wsl[s][:, k * 256 + cc * 128:k * 256 + cc * 128 + 128],
                                       rhs=cbf[:, k, :], start=(k == 0), stop=(k == 15))
                    return ins
                S.op("pe", mm, reads=[wkey(s), "cbf"], writes=[("ps", 7)])

        def ada_finish(l, secs):
            pv = ps[7][:, 0:192].rearrange("p (c t) -> p c t", t=2)
            for sec in secs:
                for t in range(2):
                    S.op("dve", lambda e, t=t, sec=sec: e.tensor_tensor(out=modp[:, l * 2 + t, sec * 16:(sec + 1) * 16], in0=pv[:, sec * 16:(sec + 1) * 16, t],
                                                                        in1=V("adab", l, sec * 16, 16), op=ALU.add),
                         reads=[("ps", 7), "vecs"], writes=[("modp", l)])
            for ni, (gn, sec) in enumerate((("n1g", 1), ("n2g", 4))):
                if sec not in secs:
                    continue
                for t in range(2):
                    S.op("dve", lambda e, ni=ni, gn=gn, sec=sec, t=t: e.scalar_tensor_tensor(
                        out=gsv[:, l * 4 + ni * 2 + t, :], in0=modp[:, l * 2 + t, sec * 16:(sec + 1) * 16], scalar=1.0, in1=V(gn, l, 0, 16),
                        op0=ALU.add, op1=ALU.mult), reads=[("modp", l), "vecs"], writes=[("gsv", l)])

        ada_q = {0: list(range(48)), 1: list(range(48))}

        def ada_some(l, n):
            for _ in range(n):
                if ada_q[l]:
                    ada_block(l, ada_q[l].pop(0))

        cur = dict(l=0)

        def modcol(t, sec, k, ll):
            return modp[:, ll * 2 + t, sec * 16 + k:sec * 16 + k + 1]

        rstd = view(C, 24576, F32, [NCOL])
        sqt = view(C, 30720, F32, [2, 512])
        ntt = view(C, 34816, F32, [2, 512])
        hx32 = view(C, 38912, F32, [2, 512])

        def norm_phase(groups, src, srckeys, ni, shsec, router=None, final=False, only=None):
            ll = cur["l"]
            for (c0, n) in groups:
                if only != "apply":
                    b = nb()
                    for k in range(16):
                        r = ring("sqt", 2)
                        if k % 2 == 0:
                            S.op("act", lambda e, k=k, r=r, c0=c0, n=n: e.activation(out=sqt[:, r, 0:n], in_=src(k, c0, n), func=AF.Square),
                                 reads=srckeys(k, c0, n), writes=[("sqt", r)])
                        else:
                            S.op("dve", lambda e, k=k, r=r, c0=c0, n=n: e.tensor_tensor(out=sqt[:, r, 0:n], in0=src(k, c0, n), in1=src(k, c0, n), op=ALU.mult),
                                 reads=srckeys(k, c0, n), writes=[("sqt", r)])
                        S.op("pe", lambda e, k=k, r=r, b=b, n=n: e.matmul(ps[b][:, 0:n], lhsT=ones[:], rhs=sqt[:, r, 0:n],
                                                                          start=(k == 0), stop=(k == 15)),
                             reads=[("sqt", r), "ones"], writes=[("ps", b)])
                    S.op("act", lambda e, b=b, c0=c0, n=n: e.activation(out=rstd[:, c0:c0 + n], in_=ps[b][:, 0:n], func=AF.Sqrt,
                                                                        bias=1e-6, scale=1.0 / D),
                         reads=[("ps", b)], writes=[("rstd", c0)])
                    S.op("dve", lambda e, c0=c0, n=n: e.reciprocal(out=rstd[:, c0:c0 + n], in_=rstd[:, c0:c0 + n]),
                         reads=[("rstd", c0)], writes=[("rstd", c0)])
                if final or only == "rstd":
                    continue
                rb = nb() if router is not None else None
                for k in range(16):
                    r = ring("ntt", 2)
                    S.op("dve", lambda e, k=k, r=r, c0=c0, n=n: e.tensor_tensor(out=ntt[:, r, 0:n], in0=src(k, c0, n), in1=rstd[:, c0:c0 + n], op=ALU.mult),
                         reads=srckeys(k, c0, n) + [("rstd", c0)], writes=[("ntt", r)])
                    if router is None:
                        for (s0, sn, kind) in segs_of(c0, n):
                            t = 0 if kind == "x" else 1
                            S.op("act", lambda e, k=k, r=r, s0=s0, sn=sn, t=t, c0=c0: e.activation(
                                out=hT[:, k, s0:s0 + sn], in_=ntt[:, r, s0 - c0:s0 - c0 + sn], func=AF.Identity,
                                scale=gsv[:, ll * 4 + ni * 2 + t, k:k + 1], bias=modcol(t, shsec, k, ll)),
                                reads=[("ntt", r), ("gsv", 0), ("gsv", 1), ("modp", 0), ("modp", 1)], writes=[("hT", k, min(c0, 1024))])
                    else:
                        r2 = ring("hx32", 2)
                        for (s0, sn, kind) in segs_of(c0, n):
                            t = 0 if kind == "x" else 1
                            S.op("act", lambda e, k=k, r=r, r2=r2, s0=s0, sn=sn, t=t, c0=c0: e.activation(
                                out=hx32[:, r2, s0 - c0:s0 - c0 + sn], in_=ntt[:, r, s0 - c0:s0 - c0 + sn], func=AF.Identity,
                                scale=gsv[:, ll * 4 + ni * 2 + t, k:k + 1], bias=modcol(t, shsec, k, ll)),
                                reads=[("ntt", r), ("gsv", 0), ("gsv", 1), ("modp", 0), ("modp", 1)], writes=[("hx32", r2)])
                        if k % 2 == 0:
                            S.op("pool", lambda e, k=k, r2=r2, c0=c0, n=n: e.tensor_copy(out=hT[:, k, c0:c0 + n], in_=hx32[:, r2, 0:n]),
                                 reads=[("hx32", r2)], writes=[("hT", k, min(c0, 1024))])
                        else:
                            S.op("act", lambda e, k=k, r2=r2, c0=c0, n=n: e.activation(out=hT[:, k, c0:c0 + n], in_=hx32[:, r2, 0:n], func=AF.Copy),
                                 reads=[("hx32", r2)], writes=[("hT", k, min(c0, 1024))])
                        S.op("pe", lambda e, k=k, r2=r2, rb=rb, n=n: e.matmul(ps[rb][0:16, 0:n], lhsT=rw[:, k, :], rhs=hx32[:, r2, 0:n],
                                                                           start=(k == 0), stop=(k == 15)),
                             reads=[("hx32", r2), "rw"], writes=[("ps", rb)])
                if router is not None:
                    router(c0, n, rb)

        def hkeys(c0):
            return [("hT", k, c0) for k in range(16)]

        def proj(s, cc, ncc, c0, n, b):
            def mm(e):
                for k in range(16):
                    o = (k * ncc + cc) * 128
                    ins = e.matmul(ps[b][:, 0:n], lhsT=wsl[s][:, o:o + 128], rhs=hT[:, k, c0:c0 + n], start=(k == 0), stop=(k == 15))
                return ins
            S.op("pe", mm, reads=[wkey(s)] + hkeys(c0), writes=[("ps", b)])

        for l in range(2):
            gin, gout = GIN[l], GOUT[l]
            if stop_after == "init":
                break
            cur["l"] = l
            if l == 0:
                def src0(k, c0, n):
                    return X9[:, k, c0 - 1408:c0 - 1408 + n] if c0 >= 1408 else xT[:, k, c0:c0 + n]

                def sk0(k, c0, n):
                    return ["X9"] if c0 >= 1408 else xkeys(k, c0, n)
                norm_phase([(0, 512), (512, 512), (1024, 384), (1408, 128)], src0, sk0, 0, 0, only="rstd")
                ada_some(0, 16)
                ada_finish(0, [0, 1])
            else:
                ada_some(1, 48)
                ada_finish(1, [0, 1, 2, 3, 4, 5])
            if l == 0:
                def src0(k, c0, n):
                    return X9[:, k, c0 - 1408:c0 - 1408 + n] if c0 >= 1408 else xT[:, k, c0:c0 + n]

                def sk0(k, c0, n):
                    return ["X9"] if c0 >= 1408 else xkeys(k, c0, n)
                norm_phase([(0, 512), (512, 512), (1024, 384), (1408, 128)], src0, sk0, 0, 0, only="apply")
            else:
                norm_phase([(0, 512), (512, 512), (1024, 384)], lambda k, c0, n: xT[:, k, c0:c0 + n], xkeys, 0, 0)
            S.op("sp", lambda e: e.dma_start(out=xsp, in_=A[:, 0:16 * NRES * 4].bitcast(F32)),
                 reads=[("xT", k, g) for k in range(16) for g in range(3)], writes=["xsp"], dma="xsp")
            S.barrier()

            if stop_after == "n1":
                break
            QT = view(A, AU, BF16, [4, NCOL])
            KTZ = view(A, AU + 12288, BF16, [2, 2, NCOL])
            VLH = view(A, AU + 24576, BF16, [12, 2, 2, 128])
            PT = view(C, 26624, BF16, [2, 5, 512])
            ropeT = view(C, 0, F32, [2, NCOL])
            maskT = view(C, 12288, F32, [2, 512])
            rt1 = view(C, 16384, F32, [2, 512])
            rt2 = view(C, 20480, F32, [2, 512])
            rdt = view(C, 24576, F32, [2, 256])
            S.op("sp", lambda e: e.dma_start(out=ropeT[:].rearrange("p a c -> p (a c)"), in_=rope_d), writes=["rope"], dma="k5")
            S.op("sp", lambda e: e.dma_start(out=maskT[:].rearrange("p a c -> p (a c)"), in_=mask_d), writes=["mask"], dma="k6")
            S.op("act", lambda e, l=l: e.activation(out=esk[:], in_=V("sink", l, 0, 4), func=AF.Exp), reads=["vecs"], writes=["esk"])
            S.op("dve", lambda e: e.memset(VLH[:].rearrange("p a b c d -> p (a b c d)"), 0.0), writes=["VLH"])
            S.op("dve", lambda e: e.memset(KTZ[:].rearrange("p a b c -> p (a b c)"), 0.0), writes=["KTZ"])
            for bi in range(6):
                s = wload(("in", l, bi))
                for (c0, n) in (gin[:2] if (l == 1 and bi < 4) else gin):
                    b1, b2 = nb(), nb()
                    proj(s, 0, 2, c0, n, b1)
                    proj(s, 1, 2, c0, n, b2)
                    r = ring("rt", 2)
                    S.op("dve", lambda e, b1=b1, r=r, c0=c0, n=n: e.tensor_tensor(out=rt1[:, r, 0:n], in0=ps[b1][:, 0:n], in1=ropeT[:, 0, c0:c0 + n], op=ALU.mult),
                         reads=[("ps", b1), "rope"], writes=[("rt1", r)])
                    S.op("dve", lambda e, b2=b2, r=r, c0=c0, n=n: e.tensor_tensor(out=rt2[:, r, 0:n], in0=ps[b2][:, 0:n], in1=ropeT[:, 1, c0:c0 + n], op=ALU.mult),
                         reads=[("ps", b2), "rope"], writes=[("rt2", r)])
                    if bi < 4:
                        S.op("dve", lambda e, bi=bi, c0=c0, r=r, n=n: e.tensor_tensor(out=QT[:, bi, c0:c0 + n], in0=rt1[:, r, 0:n], in1=rt2[:, r, 0:n], op=ALU.add),
                             reads=[("rt1", r), ("rt2", r)], writes=[("QK", bi, c0)])
                    else:
                        for hh in range(2):
                            S.op("dve", lambda e, bi=bi, c0=c0, r=r, n=n, hh=hh: e.tensor_tensor(
                                out=KTZ[hh * 64:hh * 64 + 64, bi - 4, hh, c0:c0 + n], in0=rt1[hh * 64:hh * 64 + 64, r, 0:n], in1=rt2[hh * 64:hh * 64 + 64, r, 0:n], op=ALU.add),
                                reads=[("rt1", r), ("rt2", r), "KTZ"], writes=[("QK", bi, c0, hh)])
            s = wload(("in", l, 6))
            for tb in range(3):
                b = nb()

                def mmv(e, s=s, tb=tb, b=b):
                    for tt in range(4):
                        col = (tb * 4 + tt) * 128
                        for k in range(16):
                            ins = e.matmul(ps[b][:, tt * 128:(tt + 1) * 128], lhsT=hT[:, k, col:col + 128], rhs=wsl[s][:, k * 128:(k + 1) * 128],
                                           start=(k == 0), stop=(k == 15))
                    return ins
                S.op("pe", mmv, reads=[wkey(s)] + hkeys(0) + hkeys(512) + hkeys(1024), writes=[("ps", b)])
                pvv = ps[b][:].rearrange("p (t c) -> p t c", t=4)
                for g in range(2):
                    for var in range(2):
                        S.op("act" if var == 0 else "dve",
                             (lambda e, tb=tb, g=g, var=var, pvv=pvv: e.activation(out=VLH[:, tb * 4:(tb + 1) * 4, g, var, var * 64:var * 64 + 64],
                                                                                   in_=pvv[:, :, g * 64:(g + 1) * 64], func=AF.Copy))
                             if var == 0 else
                             (lambda e, tb=tb, g=g, var=var, pvv=pvv: e.tensor_copy(out=VLH[:, tb * 4:(tb + 1) * 4, g, var, var * 64:var * 64 + 64],
                                                                                    in_=pvv[:, :, g * 64:(g + 1) * 64])),
                             reads=[("ps", b), "VLH"], writes=[("VLHw", tb, g, var)])
            vlh_keys = [("VLHw", tb, g, var) for tb in range(3) for g in range(2) for var in range(2)]
            qk_keys = [("QK", bi, c0) for bi in range(4) for (c0, n) in gin] + [("QK", bi, c0, hh) for bi in (4, 5) for (c0, n) in gin for hh in range(2)]
            if stop_after == "att1":
                break
            qtiles = []
            for t in range(9 if l == 0 else 8):
                kts = []
                if t >= 1:
                    kts.append((TCOL[t - 1], 0))
                kts.append((TCOL[t], None))
                kts.append((TCOL[t + 1], 1))
                kts += [(CTXC[0], None), (CTXC[1], None)]
                qtiles.append((TCOL[t], kts))
            if l == 0:
                for cq in CTXC:
                    qtiles.append((cq, [(CTXC[0], None), (CTXC[1], None)]))
            def att_stage_a(qc, kts, g, pb):
                for ki, (kc, mk) in enumerate(kts):
                    b = nb()

                    def mms(e, g=g, kc=kc, qc=qc, b=b):
                        for hh in range(2):
                            for cq in range(2):
                                ins = e.matmul(ps[b][:, hh * 256 + cq * 128:hh * 256 + cq * 128 + 128], lhsT=KTZ[:, g, hh, kc:kc + 128],
                                               rhs=QT[:, 2 * g + cq, qc:qc + 128], start=True, stop=True)
                        return ins
                    S.op("pe", mms, reads=qk_keys, writes=[("ps", b)])
                    S.op("act", lambda e, pb=pb, ki=ki, b=b: e.activation(out=PT[:, pb, ki, :], in_=ps[b][:], func=AF.Exp, scale=0.125),
                         reads=[("ps", b)], writes=[("PT", pb, ki)])
                    if mk is not None:
                        S.op("dve", lambda e, pb=pb, ki=ki, mk=mk: e.tensor_tensor(out=PT[:, pb, ki, :], in0=PT[:, pb, ki, :], in1=maskT[:, mk, :], op=ALU.mult),
                             reads=[("PT", pb, ki), "mask"], writes=[("PT", pb, ki)])

            def att_stage_b(qc, kts, g, pb):
                bo, bd = nb(), nb()
                nk = len(kts)

                def mmo(e):
                    for ki, (kc, mk) in enumerate(kts):
                        ct = kc // 128
                        e.matmul(ps[bo][:, 0:256], lhsT=VLH[:, ct, g, 0, :], rhs=PT[:, pb, ki, 0:256], start=(ki == 0), stop=False)
                        ins = e.matmul(ps[bo][:, 0:256], lhsT=VLH[:, ct, g, 1, :], rhs=PT[:, pb, ki, 256:512], start=False, stop=(ki == nk - 1))
                    return ins

                def mmd(e):
                    for ki in range(nk):
                        e.matmul(ps[bd][:, 0:256], lhsT=oneslh[:, 0, :], rhs=PT[:, pb, ki, 0:256], start=(ki == 0), stop=False)
                        ins = e.matmul(ps[bd][:, 0:256], lhsT=oneslh[:, 1, :], rhs=PT[:, pb, ki, 256:512], start=False, stop=(ki == nk - 1))
                    return ins
                ptk = [("PT", pb, ki) for ki in range(nk)]
                S.op("pe", mmo, reads=ptk + vlh_keys, writes=[("ps", bo)])
                S.op("pe", mmd, reads=ptk + ["oneslh"], writes=[("ps", bd)])
                rr = ring("rdt", 2)
                for cc in range(2):
                    S.op("dve", lambda e, cc=cc: e.tensor_single_scalar(out=rdt[:, rr, cc * 128:(cc + 1) * 128], in_=ps[bd][:, cc * 128:(cc + 1) * 128],
                                                                        scalar=esk[:, 2 * g + cc:2 * g + cc + 1], op=ALU.add),
                         reads=[("ps", bd), "esk"], writes=[("rdt", rr, cc)])
                S.op("dve", lambda e: e.reciprocal(out=rdt[:, rr, :], in_=rdt[:, rr, :]),
                     reads=[("rdt", rr, 0), ("rdt", rr, 1)], writes=[("rdt", rr, 0), ("rdt", rr, 1)])
                S.op("dve", lambda e: e.tensor_tensor(
                    out=ybr[:, 2 * g:2 * g + 2, qc:qc + 128], in0=ps[bo][:, 0:256].rearrange("p (a c) -> p a c", a=2),
                    in1=rdt[:, rr, :].rearrange("p (a c) -> p a c", a=2), op=ALU.mult),
                    reads=[("ps", bo), ("rdt", rr, 0), ("rdt", rr, 1)], writes=[("ybr", 0, qc)])

            pairs = [(qc, kts, g) for (qc, kts) in qtiles for g in range(2)]
            for i, (qc, kts, g) in enumerate(pairs):
                if l == 0 and g == 0:
                    ada_some(0, 3)
                att_stage_a(qc, kts, g, i % 2)
                if i >= 1:
                    pq, pk, pg = pairs[i - 1]
                    att_stage_b(pq, pk, pg, (i - 1) % 2)
            pq, pk, pg = pairs[-1]
            att_stage_b(pq, pk, pg, (len(pairs) - 1) % 2)
            if l == 0:
                ada_some(0, 48)
                ada_finish(0, [2, 3, 4, 5])
            S.barrier()

            if stop_after == "att":
                break
            UG = view(A, AU, BF16, [4, NCOL])
            gsq = view(A, AU + 12288, F32, [2, 512])
            gin_ = view(A, AU + 16384, F32, [2, 512])
            gsg = view(A, AU + 20480, F32, [2, 512])
            cen = view(A, AU + 24576, F32, [2, 512])
            sqv = view(A, AU + 28672, F32, [2, 512])
            VG = view(C, 0, F32, [12, 512])
            VTM = view(C, 24576, BF16, [12, 512])
            wsT = view(C, 36864, BF16, [4, 128])
            bsb = view(C, 37888, F32, [4, 128])
            lngb = view(C, 39936, F32, [512])
            lnbb = view(C, 41984, F32, [512])
            sgt = view(C, 44032, F32, [2, 128])
            wsTf = view(A, AU + 32768, F32, [512])
            S.op("sp", lambda e, l=l: e.dma_start(out=wsTf[:], in_=sguw_d[l * 128:(l + 1) * 128, :]), writes=["wsTf"], dma="k7")
            S.op("act", lambda e: e.activation(out=wsT[:].rearrange("p g c -> p (g c)"), in_=wsTf[:], func=AF.Copy), reads=["wsTf"], writes=["wsT"])
            S.op("sp", lambda e, l=l: e.dma_start(out=lngb[:], in_=rowv_d[l, 0:512].partition_broadcast(128)), writes=["lngb"], dma="k8")
            S.op("sp", lambda e, l=l: e.dma_start(out=lnbb[:], in_=rowv_d[l, 512:1024].partition_broadcast(128)), writes=["lnbb"], dma="k9")
            S.op("sp", lambda e, l=l: e.dma_start(out=bsb[:].rearrange("p g c -> p (g c)"), in_=rowv_d[l, 1024:1536].partition_broadcast(128)), writes=["bsb"], dma="k10")

            def gelu_ops(src_ap, src_keys, n, dst_ap, dst_keys):
                r = ring("gelu", 2)
                S.op("act", lambda e: e.activation(out=gsq[:, r, 0:n], in_=src_ap, func=AF.Square), reads=src_keys, writes=[("gsq", r)])
                S.op("dve", lambda e: e.tensor_scalar(out=gin_[:, r, 0:n], in0=gsq[:, r, 0:n], scalar1=0.044715, scalar2=1.0, op0=ALU.mult, op1=ALU.add),
                     reads=[("gsq", r)], writes=[("gin", r)])
                S.op("dve", lambda e: e.tensor_tensor(out=gin_[:, r, 0:n], in0=gin_[:, r, 0:n], in1=src_ap, op=ALU.mult),
                     reads=[("gin", r)] + src_keys, writes=[("gin", r)])
                S.op("act", lambda e: e.activation(out=gsg[:, r, 0:n], in_=gin_[:, r, 0:n], func=AF.Sigmoid, scale=1.5957691216057308),
                     reads=[("gin", r)], writes=[("gsg", r)])
                S.op("dve", lambda e: e.tensor_tensor(out=dst_ap, in0=gsg[:, r, 0:n], in1=src_ap, op=ALU.mult),
                     reads=[("gsg", r)] + src_keys, writes=dst_keys)

            for bi in range(2):
                s = wload(("in", l, 7 + bi))
                for cc in range(2):
                    j = bi * 2 + cc
                    for (c0, n) in (gin[:2] if l == 1 else gin):
                        b = nb()
                        proj(s, cc, 2, c0, n, b)
                        gelu_ops(ps[b][:, 0:n], [("ps", b)], n, UG[:, j, c0:c0 + n], [("UG", j, c0)])
            ntile_v = 11 if l == 0 else 8
            vcols = [128 * i for i in range(11)] if l == 0 else [128 * i for i in range(8)]
            for hb in range(2):
                s = wload(("in", l, 9 + hb))
                for ti, col in enumerate(vcols):
                    b = nb()

                    def mmsv(e, s=s, col=col, b=b):
                        for k in range(16):
                            ins = e.matmul(ps[b][:, 0:256], lhsT=hT[:, k, col:col + 128], rhs=wsl[s][:, k * 256:(k + 1) * 256],
                                           start=(k == 0), stop=(k == 15))
                        return ins
                    S.op("pe", mmsv, reads=[wkey(s)] + hkeys((col // 512) * 512), writes=[("ps", b)])
                    gelu_ops(ps[b][:, 0:256], [("ps", b)], 256, VG[:, ti, hb * 256:(hb + 1) * 256], [("VG", ti, hb)])
            for ti, col in enumerate(vcols):
                if l == 0:
                    ada_some(1, 2)
                vk = [("VG", ti, 0), ("VG", ti, 1)]
                r = ring("sgt", 2)
                S.op("dve", lambda e, ti=ti, r=r: e.tensor_reduce(out=sgt[:, r, 0:1], in_=VG[:, ti, :], axis=AX.X, op=ALU.add), reads=vk, writes=[("sgt", r)])
                S.op("dve", lambda e, r=r: e.tensor_single_scalar(out=sgt[:, r, 1:2], in_=sgt[:, r, 0:1], scalar=1.0 / 512, op=ALU.mult),
                     reads=[("sgt", r)], writes=[("sgt", r)])
                S.op("dve", lambda e, ti=ti, r=r: e.tensor_single_scalar(out=cen[:, r, :], in_=VG[:, ti, :], scalar=sgt[:, r, 1:2], op=ALU.subtract),
                     reads=vk + [("sgt", r)], writes=[("cen", r)])
                S.op("dve", lambda e, r=r: e.tensor_tensor(out=sqv[:, r, :], in0=cen[:, r, :], in1=cen[:, r, :], op=ALU.mult), reads=[("cen", r)], writes=[("sqv", r)])
                S.op("dve", lambda e, r=r: e.tensor_reduce(out=sgt[:, r, 2:3], in_=sqv[:, r, :], axis=AX.X, op=ALU.add), reads=[("sqv", r), ("sgt", r)], writes=[("sgt", r)])
                S.op("act", lambda e, r=r: e.activation(out=sgt[:, r, 3:4], in_=sgt[:, r, 2:3], func=AF.Sqrt, bias=1e-5, scale=1.0 / 512), reads=[("sgt", r)], writes=[("sgt", r)])
                S.op("dve", lambda e, r=r: e.reciprocal(out=sgt[:, r, 4:5], in_=sgt[:, r, 3:4]), reads=[("sgt", r)], writes=[("sgt", r)])
                S.op("dve", lambda e, r=r: e.scalar_tensor_tensor(out=cen[:, r, :], in0=cen[:, r, :], scalar=sgt[:, r, 4:5], in1=lngb[:], op0=ALU.mult, op1=ALU.mult),
                     reads=[("cen", r), ("sgt", r), "lngb"], writes=[("cen", r)])
                S.op("dve", lambda e, ti=ti, r=r: e.tensor_tensor(out=VTM[:, ti, :], in0=cen[:, r, :], in1=lnbb[:], op=ALU.add),
                     reads=[("cen", r), "lnbb"], writes=[("VTM", ti)])
                b = nb()

                def mmsp(e, ti=ti, b=b):
                    for g in range(4):
                        ins = e.matmul(ps[b][:, g * 128:(g + 1) * 128], lhsT=VTM[:, ti, g * 128:(g + 1) * 128], rhs=wsT[:, g, :], start=True, stop=True)
                    return ins
                S.op("pe", mmsp, reads=[("VTM", ti), "wsT"], writes=[("ps", b)])
                r2 = ring("spt", 2)
                S.op("dve", lambda e, b=b, r2=r2: e.tensor_tensor(out=gsq[:, r2, :].rearrange("p (g c) -> p g c", g=4), in0=ps[b][:].rearrange("p (g c) -> p g c", g=4),
                                                                  in1=bsb[:], op=ALU.add), reads=[("ps", b), "bsb"], writes=[("gsq", r2)])
                S.op("dve", lambda e, r2=r2, col=col: e.tensor_tensor(out=ybr[:, 4:8, col:col + 128], in0=gsq[:, r2, :].rearrange("p (g c) -> p g c", g=4),
                                                                     in1=UG[:, :, col:col + 128], op=ALU.mult),
                     reads=[("gsq", r2)] + [("UG", j, (col // 512) * 512) for j in range(4)], writes=[("ybr", 1, col)])
            S.barrier()

            if stop_after == "sgu":
                break
            def sc_set(j):
                if j % 2 == 0:
                    return (view(C, 0, F32, [NCOL]), view(C, 6144, F32, [NCOL]), view(C, 12288, F32, [CONVL]), view(C, 18624, F32, [CONVL]))
                return (view(A, AU, F32, [NCOL]), view(A, AU + 6144, F32, [NCOL]), view(A, AU + 12288, F32, [CONVL]), view(A, AU + 18624, F32, [CONVL]))
            for j in range(2):
                zp = sc_set(j)[2]
                S.op("dve", lambda e, zp=zp: e.memset(zp[:], 0.0), writes=[("ZP", j)])
            sc_order = ["B0", "C0", "x0", "B1", "C1", "x1", "B2", "C2", "x2", "B3", "C3", "x3"]
            for ci, nm in enumerate(sc_order):
                if ci % 2 == 0:
                    if l == 0:
                        ada_some(1, 2)
                    s = wload(("in", l, 11 + ci // 2))
                cc = ci % 2
                j = int(nm[1])
                Bs, Cs, ZP, ACC = sc_set(j)
                for (c0, n) in (gin[:2] if (l == 1 and nm[0] == "B") else gin):
                    b = nb()
                    proj(s, cc, 2, c0, n, b)
                    if nm[0] == "B":
                        S.op("act", lambda e, Bs=Bs, b=b, c0=c0, n=n: e.activation(out=Bs[:, c0:c0 + n], in_=ps[b][:, 0:n], func=AF.Copy),
                             reads=[("ps", b)], writes=[("Bs", j % 2, c0)])
                    elif nm[0] == "C":
                        S.op("act", lambda e, Cs=Cs, b=b, c0=c0, n=n: e.activation(out=Cs[:, c0:c0 + n], in_=ps[b][:, 0:n], func=AF.Copy),
                             reads=[("ps", b)], writes=[("Cs", j % 2, c0)])
                    else:
                        for (q0, qn, pos) in pieces_of(c0, n):
                            S.op("dve", lambda e, ZP=ZP, Cs=Cs, b=b, c0=c0, q0=q0, qn=qn, pos=pos: e.tensor_tensor(
                                out=ZP[:, pos:pos + qn], in0=ps[b][:, q0 - c0:q0 - c0 + qn], in1=Cs[:, q0:q0 + qn], op=ALU.mult),
                                reads=[("ps", b), ("Cs", j % 2, c0), ("ZP", j % 2)], writes=[("ZPw", j % 2, c0)])
                if nm[0] == "x":
                    zk = [("ZPw", j % 2, c0) for (c0, n) in gin] + [("ZP", j % 2)]
                    L0, L1 = 16, CONVL - 16
                    S.op("dve", lambda e, ACC=ACC, ZP=ZP, j=j, l=l: e.tensor_single_scalar(out=ACC[:, L0:L1], in_=ZP[:, L0 - 1:L1 - 1], scalar=V("scw", l, j * 3 + 0), op=ALU.mult),
                         reads=zk + ["vecs"], writes=[("ACC", j % 2)])
                    for tap in (1, 2):
                        S.op("dve", lambda e, ACC=ACC, ZP=ZP, j=j, l=l, tap=tap: e.scalar_tensor_tensor(
                            out=ACC[:, L0:L1], in0=ZP[:, L0 - 1 + tap:L1 - 1 + tap], scalar=V("scw", l, j * 3 + tap), in1=ACC[:, L0:L1], op0=ALU.mult, op1=ALU.add),
                            reads=zk + [("ACC", j % 2), "vecs"], writes=[("ACC", j % 2)])
                    for (q0, qn, pos) in pieces_of(0, NRES if l == 0 else 1024):
                        S.op("dve", lambda e, ACC=ACC, Bs=Bs, j=j, q0=q0, qn=qn, pos=pos: e.tensor_tensor(
                            out=ybr[:, 8 + j, q0:q0 + qn], in0=ACC[:, pos:pos + qn], in1=Bs[:, q0:q0 + qn], op=ALU.mult),
                            reads=[("ACC", j % 2)] + [("Bs", j % 2, c0) for (c0, n) in gin], writes=[("ybr", 2, j, q0)])
            S.barrier()

            if stop_after == "sc":
                break
            ZC = view(A, AU, F32, [4, CONVL])
            ZP2 = view(C, 0, BF16, [2, CONVL])
            sgm = view(C, 6336, F32, [2, 512])
            lm = view(C, 10432, F32, [512])
            lmsq = view(C, 12480, F32, [512])
            lrs = view(C, 14528, F32, [512])
            lcen = view(C, 16576, F32, [2, 512])
            lsq = view(C, 20672, F32, [2, 512])
            DG = view(C, 24768, BF16, [2, 31, 128])
            lnp = [(16, 512, 0), (528, 512, 512)] + ([(1040, 128, 1024), (1312, 256, 1152)] if l == 0 else [])
            for r in range(2):
                S.op("dve", lambda e, r=r: e.memset(ZP2[:, r, :], 0.0), writes=[("ZP2", r)])
            for j in range(4):
                s = wload(("in", l, 17 + j))
                r = j % 2
                for tap in range(31):
                    if tap % 2 == 0:
                        S.op("act", lambda e, j=j, r=r, l=l, tap=tap: e.activation(out=DG[:, r, tap, :], in_=identb[:], func=AF.Identity, scale=V("cdw", l, j * 31 + tap)),
                             reads=["identb", "vecs"], writes=[("DG", r, tap)])
                    else:
                        S.op("dve", lambda e, j=j, r=r, l=l, tap=tap: e.tensor_single_scalar(out=DG[:, r, tap, :], in_=identb[:], scalar=V("cdw", l, j * 31 + tap), op=ALU.mult),
                             reads=["identb", "vecs"], writes=[("DG", r, tap)])
                for (c0, n) in gin:
                    ba, bb = nb(), nb()
                    proj(s, 0, 2, c0, n, ba)
                    proj(s, 1, 2, c0, n, bb)
                    rs = ring("sgm", 2)
                    S.op("act", lambda e, bb=bb, rs=rs, n=n: e.activation(out=sgm[:, rs, 0:n], in_=ps[bb][:, 0:n], func=AF.Sigmoid),
                         reads=[("ps", bb)], writes=[("sgm", rs)])
                    for (q0, qn, pos) in pieces_of(c0, n):
                        S.op("dve", lambda e, ba=ba, rs=rs, r=r, c0=c0, q0=q0, qn=qn, pos=pos: e.tensor_tensor(
                            out=ZP2[:, r, pos:pos + qn], in0=ps[ba][:, q0 - c0:q0 - c0 + qn], in1=sgm[:, rs, q0 - c0:q0 - c0 + qn], op=ALU.mult),
                            reads=[("ps", ba), ("sgm", rs), ("ZP2", r)], writes=[("ZP2w", r, c0)])
                if l == 0:
                    ada_some(1, 9)
                zk = [("ZP2w", r, c0) for (c0, n) in gin] + [("ZP2", r)]
                dgk = [("DG", r, tap) for tap in range(31)]
                for (pos, n, col) in lnp:
                    b = nb()

                    def mmc(e, r=r, pos=pos, n=n, b=b):
                        for tap in range(31):
                            ins = e.matmul(ps[b][:, 0:n], lhsT=DG[:, r, tap, :], rhs=ZP2[:, r, pos - 15 + tap:pos - 15 + tap + n], start=(tap == 0), stop=(tap == 30))
                        return ins
                    S.op("pe", mmc, reads=zk + dgk, writes=[("ps", b)])
                    S.op("act", lambda e, j=j, l=l, pos=pos, n=n, b=b: e.activation(out=ZC[:, j, pos:pos + n], in_=ps[b][:, 0:n], func=AF.Identity, bias=V("cdb", l, j), scale=1.0),
                         reads=[("ps", b), "vecs"], writes=[("ZC", j)])
            for (pos, n, col) in lnp:
                b1, b2 = nb(), nb()
                S.op("pe", lambda e, b1=b1, pos=pos, n=n: [e.matmul(ps[b1][:, 0:n], lhsT=ones[:], rhs=ZC[:, j, pos:pos + n], start=(j == 0), stop=(j == 3)) for j in range(4)][-1],
                     reads=[("ZC", j) for j in range(4)] + ["ones"], writes=[("ps", b1)])
                for j in range(4):
                    r = ring("lsq", 2)
                    S.op("act", lambda e, j=j, r=r, pos=pos, n=n: e.activation(out=lsq[:, r, 0:n], in_=ZC[:, j, pos:pos + n], func=AF.Square), reads=[("ZC", j)], writes=[("lsq", r)])
                    S.op("pe", lambda e, j=j, r=r, b2=b2, n=n: e.matmul(ps[b2][:, 0:n], lhsT=ones[:], rhs=lsq[:, r, 0:n], start=(j == 0), stop=(j == 3)),
                         reads=[("lsq", r), "ones"], writes=[("ps", b2)])
                S.op("dve", lambda e, b1=b1, n=n: e.tensor_single_scalar(out=lm[:, 0:n], in_=ps[b1][:, 0:n], scalar=1.0 / 512, op=ALU.mult), reads=[("ps", b1)], writes=["lm"])
                S.op("dve", lambda e, n=n: e.tensor_tensor(out=lmsq[:, 0:n], in0=lm[:, 0:n], in1=lm[:, 0:n], op=ALU.mult), reads=["lm"], writes=["lmsq"])
                S.op("dve", lambda e, b2=b2, n=n: e.scalar_tensor_tensor(out=lrs[:, 0:n], in0=ps[b2][:, 0:n], scalar=1.0 / 512, in1=lmsq[:, 0:n], op0=ALU.mult, op1=ALU.subtract),
                     reads=[("ps", b2), "lmsq"], writes=["lrs"])
                S.op("act", lambda e, n=n: e.activation(out=lrs[:, 0:n], in_=lrs[:, 0:n], func=AF.Sqrt, bias=1e-5, scale=1.0), reads=["lrs"], writes=["lrs"])
                S.op("dve", lambda e, n=n: e.reciprocal(out=lrs[:, 0:n], in_=lrs[:, 0:n]), reads=["lrs"], writes=["lrs"])
                for j in range(4):
                    r = ring("lcen", 2)
                    S.op("dve", lambda e, j=j, r=r, pos=pos, n=n: e.tensor_tensor(out=lcen[:, r, 0:n], in0=ZC[:, j, pos:pos + n], in1=lm[:, 0:n], op=ALU.subtract),
                         reads=[("ZC", j), "lm"], writes=[("lcen", r)])
                    S.op("dve", lambda e, r=r, n=n: e.tensor_tensor(out=lcen[:, r, 0:n], in0=lcen[:, r, 0:n], in1=lrs[:, 0:n], op=ALU.mult),
                         reads=[("lcen", r), "lrs"], writes=[("lcen", r)])
                    S.op("act", lambda e, j=j, r=r, col=col, n=n, l=l: e.activation(out=ybr[:, 12 + j, col:col + n], in_=lcen[:, r, 0:n], func=AF.Silu,
                                                                                 scale=V("clg", l, j), bias=V("clb", l, j)),
                         reads=[("lcen", r), "vecs"], writes=[("ybr", 3, j, col)])
            S.barrier()

            if stop_after == "conf":
                break
            sgr = view(A, AU, F32, [12, 512])
            accf = view(A, AU + 24576, F32, [2, 512])
            tmpm = view(A, AU + 28672, F32, [4, 512])
            for dc in range(16):
                for half in range(2):
                    sg = wload(("g", l, dc, half))
                    for gi_, (c0, n) in enumerate(gout):
                        for cc in range(2):
                            i = half * 2 + cc
                            b = nb()
                            proj(sg, cc, 2, c0, n, b)
                            r = i * 3 + gi_
                            S.op("act", lambda e, b=b, r=r, n=n: e.activation(out=sgr[:, r, 0:n], in_=ps[b][:, 0:n], func=AF.Sigmoid), reads=[("ps", b)], writes=[("sgr", r)])
                sb_ = wload(("wb", l, dc))
                for gi_, (c0, n) in enumerate(gout):
                    pbk = []
                    for i in range(4):
                        b = nb()

                        def mmp(e, i=i, b=b, c0=c0, n=n, sb_=sb_):
                            for kc in range(4):
                                o = (i * 4 + kc) * 128
                                ins = e.matmul(ps[b][:, 0:n], lhsT=wsl[sb_][:, o:o + 128], rhs=ybr[:, i * 4 + kc, c0:c0 + n], start=(kc == 0), stop=(kc == 3))
                            return ins
                        S.op("pe", mmp, reads=[wkey(sb_)], writes=[("ps", b)])
                        pbk.append(b)
                    sgk = [i * 3 + gi_ for i in range(4)]
                    ra = ring("accf", 2)
                    S.op("dve", lambda e, ra=ra, n=n, b=pbk[0], r=sgk[0]: e.tensor_tensor(out=accf[:, ra, 0:n], in0=ps[b][:, 0:n], in1=sgr[:, r, 0:n], op=ALU.mult),
                         reads=[("ps", pbk[0]), ("sgr", sgk[0])], writes=[("accf", ra)])
                    for i in range(1, 4):
                        rt = ring("tmpm", 4)
                        S.op("dve", lambda e, rt=rt, n=n, b=pbk[i], r=sgk[i]: e.tensor_tensor(out=tmpm[:, rt, 0:n], in0=ps[b][:, 0:n], in1=sgr[:, r, 0:n], op=ALU.mult),
                             reads=[("ps", pbk[i]), ("sgr", sgk[i])], writes=[("tmpm", rt)])
                        if i < 3:
                            S.op("dve", lambda e, rt=rt, ra=ra, n=n: e.tensor_tensor(out=accf[:, ra, 0:n], in0=accf[:, ra, 0:n], in1=tmpm[:, rt, 0:n], op=ALU.add),
                                 reads=[("accf", ra), ("tmpm", rt)], writes=[("accf", ra)])
                        else:
                            S.op("dve", lambda e, rt=rt, ra=ra, n=n, dc=dc, c0=c0: e.tensor_tensor(out=merged[:, dc, c0:c0 + n], in0=accf[:, ra, 0:n], in1=tmpm[:, rt, 0:n], op=ALU.add),
                                 reads=[("accf", ra), ("tmpm", rt)], writes=[("mg", dc, c0)])
            S.barrier()

            if stop_after == "mg":
                break
            stg = hT[:].rearrange("p k c -> p (k c)")[:, 0:3072].bitcast(F32).rearrange("p (a c) -> p a c", a=3)
            xspv = xsp.rearrange("p (k c) -> p k c", k=16)
            for pr in range(8):
                s = wload(("wo", l, pr))
                for cc in range(2):
                    d2 = pr * 2 + cc
                    for (c0, n) in gout:
                        r = ring("stg", 3)
                        S.op("sp", lambda e, r=r, d2=d2, c0=c0, n=n: e.dma_start(out=stg[:, r, 0:n], in_=xspv[:, d2, c0:c0 + n]),
                             reads=["xsp"], writes=[("stg", r)], dma="stg%d" % r)
                        b = nb()

                        def mmw(e, s=s, cc=cc, b=b, c0=c0, n=n):
                            for k in range(16):
                                o = (k * 2 + cc) * 128
                                ins = e.matmul(ps[b][:, 0:n], lhsT=wsl[s][:, o:o + 128], rhs=merged[:, k, c0:c0 + n], start=(k == 0), stop=(k == 15))
                            return ins
                        S.op("pe", mmw, reads=[wkey(s)] + [("mg", k, c0) for k in range(16)], writes=[("ps", b)])
                        for (s0, sn, kind) in segs_of(c0, n):
                            t = 0 if kind == "x" else 1
                            S.op("dve", lambda e, b=b, r=r, d2=d2, c0=c0, s0=s0, sn=sn, mc=modcol(t, 2, d2, l): e.scalar_tensor_tensor(
                                out=xT[:, d2, s0:s0 + sn], in0=ps[b][:, s0 - c0:s0 - c0 + sn], scalar=mc, in1=stg[:, r, s0 - c0:s0 - c0 + sn],
                                op0=ALU.mult, op1=ALU.add), reads=[("ps", b), ("stg", r), ("modp", 0), ("modp", 1)], writes=xkeys(d2, c0, n))
            S.barrier()
            if stop_after == "mix%d" % l:
                break

            lgT = view(C, 0, F32, [NRES])
            LG = view(C, 5632, F32, [11, 16])
            RT = view(C, 6400, F32, [20, 16])
            COMB = view(C, 7680, F32, [11, 16])
            cTs = view(C, 8384, F32, [NRES])

            def router(c0, n, rb):
                S.op("act", lambda e: e.activation(out=lgT[0:16, c0:c0 + n], in_=ps[rb][0:16, 0:n], func=AF.Copy), reads=[("ps", rb)], writes=[("lgT", c0)])
                b = nb()

                def trl(e):
                    for tt in range(n // 128):
                        ins = e.transpose(out=ps[b][:, tt * 16:(tt + 1) * 16], in_=lgT[0:16, c0 + tt * 128:c0 + (tt + 1) * 128], identity=ident[0:16, 0:16])
                    return ins
                S.op("pe", trl, reads=[("lgT", c0), "ident"], writes=[("ps", b)])
                t0 = c0 // 128
                nt = n // 128
                S.op("act", lambda e: e.activation(out=LG[:, t0:t0 + nt, :], in_=ps[b][:, 0:nt * 16].rearrange("p (t c) -> p t c", t=nt), func=AF.Copy),
                     reads=[("ps", b)], writes=[("LG", c0)])
            norm_phase(gout, lambda k, c0, n: xT[:, k, c0:c0 + n], xkeys, 1, 3, router=router)
            ntl = 11 if l == 0 else 8

            def routing_block(nt, gout):
                RB = view(C, 14016, F32, [9, 11, 16])
                RS = view(C, 14016 + 6336, F32, [9, 11, 4])
                lk = [("LG", c0) for (c0, n) in gout]
                T = LG[:, 0:nt, :]
                big = lambda i: RB[:, i, 0:nt, :]
                smv = lambda i, w: RS[:, i, 0:nt, 0:w]
                sm1 = lambda i: RS[:, i, 0:nt, 0]
                bc16 = lambda i: RS[:, i, 0:nt, 0:1].to_broadcast([128, nt, 16])
                g44 = lambda ap: ap.rearrange("p t (g c) -> p t g c", g=4)
                bk = lambda i: ("RB", i)
                sk = lambda i: ("RS", i)

                def dv(fn, rd, wr):
                    S.op("dve", fn, reads=rd, writes=wr)
                dv(lambda e: e.tensor_reduce(out=sm1(0), in_=T, axis=AX.X, op=ALU.max), lk, [sk(0)])
                dv(lambda e: e.tensor_tensor(out=big(0), in0=T, in1=bc16(0), op=ALU.subtract), lk + [sk(0)], [bk(0)])
                S.op("act", lambda e: e.activation(out=big(0), in_=big(0), func=AF.Exp), reads=[bk(0)], writes=[bk(0)])
                dv(lambda e: e.tensor_reduce(out=sm1(1), in_=big(0), axis=AX.X, op=ALU.add), [bk(0)], [sk(1)])
                dv(lambda e: e.reciprocal(out=sm1(1), in_=sm1(1)), [sk(1)], [sk(1)])
                dv(lambda e: e.tensor_tensor(out=big(1), in0=big(0), in1=bc16(1), op=ALU.mult), [bk(0), sk(1)], [bk(1)])
                dv(lambda e: e.tensor_tensor(out=big(2), in0=big(1), in1=rbb[:].unsqueeze(1).to_broadcast([128, nt, 16]), op=ALU.add), [bk(1), "rbb"], [bk(2)])
                dv(lambda e: e.tensor_reduce(out=smv(2, 4), in_=g44(big(2)), axis=AX.X, op=ALU.max), [bk(2)], [sk(2)])
                dv(lambda e: e.tensor_tensor(out=g44(big(3)), in0=g44(big(2)), in1=smv(2, 4).unsqueeze(3).to_broadcast([128, nt, 4, 4]), op=ALU.is_equal), [bk(2), sk(2)], [bk(3)])
                dv(lambda e: e.scalar_tensor_tensor(out=big(3), in0=big(3), scalar=NEG, in1=big(2), op0=ALU.mult, op1=ALU.add), [bk(3), bk(2)], [bk(3)])
                dv(lambda e: e.tensor_reduce(out=smv(3, 4), in_=g44(big(3)), axis=AX.X, op=ALU.max), [bk(3)], [sk(3)])
                dv(lambda e: e.tensor_tensor(out=smv(3, 4), in0=smv(3, 4), in1=smv(2, 4), op=ALU.add), [sk(3), sk(2)], [sk(3)])
                dv(lambda e: e.tensor_reduce(out=sm1(4), in_=smv(3, 4), axis=AX.X, op=ALU.max), [sk(3)], [sk(4)])
                dv(lambda e: e.tensor_tensor(out=smv(5, 4), in0=smv(3, 4), in1=RS[:, 4, 0:nt, 0:1].to_broadcast([128, nt, 4]), op=ALU.is_equal), [sk(3), sk(4)], [sk(5)])
                dv(lambda e: e.tensor_scalar(out=smv(5, 4), in0=smv(5, 4), scalar1=1.0, scalar2=-NEG, op0=ALU.subtract, op1=ALU.mult), [sk(5)], [sk(5)])
                dv(lambda e: e.tensor_tensor(out=g44(big(4)), in0=g44(big(2)), in1=smv(5, 4).unsqueeze(3).to_broadcast([128, nt, 4, 4]), op=ALU.add), [bk(2), sk(5)], [bk(4)])
                dv(lambda e: e.tensor_reduce(out=sm1(6), in_=big(4), axis=AX.X, op=ALU.max), [bk(4)], [sk(6)])
                dv(lambda e: e.tensor_tensor(out=big(5), in0=big(4), in1=bc16(6), op=ALU.is_equal), [bk(4), sk(6)], [bk(5)])
                dv(lambda e: e.scalar_tensor_tensor(out=big(6), in0=big(5), scalar=NEG, in1=big(4), op0=ALU.mult, op1=ALU.add), [bk(5), bk(4)], [bk(6)])
                dv(lambda e: e.tensor_reduce(out=sm1(7), in_=big(6), axis=AX.X, op=ALU.max), [bk(6)], [sk(7)])
                dv(lambda e: e.tensor_tensor(out=big(7), in0=big(6), in1=bc16(7), op=ALU.is_equal), [bk(6), sk(7)], [bk(7)])
                dv(lambda e: e.tensor_tensor(out=big(7), in0=big(7), in1=big(5), op=ALU.add), [bk(7), bk(5)], [bk(7)])
                dv(lambda e: e.tensor_tensor(out=big(8), in0=big(7), in1=big(1), op=ALU.mult), [bk(7), bk(1)], [bk(8)])
                dv(lambda e: e.tensor_reduce(out=sm1(8), in_=big(8), axis=AX.X, op=ALU.add), [bk(8)], [sk(8)])
                dv(lambda e: e.reciprocal(out=sm1(8), in_=sm1(8)), [sk(8)], [sk(8)])
                dv(lambda e: e.tensor_tensor(out=COMB[:, 0:nt, :], in0=big(8), in1=bc16(8), op=ALU.mult), [bk(8), sk(8)], [("COMB", ti) for ti in range(nt)])

            routing_block(ntl, gout)
            for (c0, n) in gout:
                b = nb()
                t0 = c0 // 128

                def trc(e, b=b, t0=t0, n=n):
                    for tt in range(n // 128):
                        ins = e.transpose(out=ps[b][0:16, tt * 128:(tt + 1) * 128], in_=COMB[:, t0 + tt, :], identity=ident[:])
                    return ins
                S.op("pe", trc, reads=[("COMB", t0 + tt) for tt in range(n // 128)] + ["ident"], writes=[("ps", b)])
                S.op("act", lambda e, b=b, c0=c0, n=n: e.activation(out=cTs[0:16, c0:c0 + n], in_=ps[b][0:16, 0:n], func=AF.Copy), reads=[("ps", b)], writes=[("cTs", c0)])
            ntok = gout[-1][0] + gout[-1][1]
            S.op("sp", lambda e, ntok=ntok: e.dma_start(out=ctd[:, 0:ntok], in_=cTs[0:16, 0:ntok]), reads=[("cTs", c0) for (c0, n) in gout], writes=["ctd"], dma="ctd")
            S.barrier()

            actT = view(C, 0, BF16, [8, NRES])
            cbc = view(C, 22528, F32, [2, NRES])
            sat = view(C, 33792, F32, [2, 512])
            tbt = view(C, 37888, F32, [2, 512])
            for ex in range(16):
                cb = ex % 2
                S.op("sp", lambda e, ex=ex, cb=cb, ntok=ntok: e.dma_start(out=cbc[:, cb, 0:ntok], in_=ctd[ex, 0:ntok].partition_broadcast(128)),
                     reads=["ctd"], writes=[("cbc", cb)], dma="cbc%d" % cb)
                for fc in range(8):
                    s = wload(("up", l, ex, fc))
                    for (c0, n) in gout:
                        ba, bb = nb(), nb()
                        proj(s, 0, 2, c0, n, ba)
                        proj(s, 1, 2, c0, n, bb)
                        r = ring("sat", 2)
                        S.op("act", lambda e, ba=ba, r=r, n=n: e.activation(out=sat[:, r, 0:n], in_=ps[ba][:, 0:n], func=AF.Silu), reads=[("ps", ba)], writes=[("sat", r)])
                        S.op("dve", lambda e, bb=bb, r=r, n=n: e.tensor_tensor(out=tbt[:, r, 0:n], in0=ps[bb][:, 0:n], in1=sat[:, r, 0:n], op=ALU.mult),
                             reads=[("ps", bb), ("sat", r)], writes=[("tbt", r)])
                        S.op("dve", lambda e, r=r, fc=fc, cb=cb, c0=c0, n=n: e.tensor_tensor(out=actT[:, fc, c0:c0 + n], in0=tbt[:, r, 0:n], in1=cbc[:, cb, c0:c0 + n], op=ALU.mult),
                             reads=[("tbt", r), ("cbc", cb)], writes=[("actT", fc, c0)])
                for db in range(4):
                    s = wload(("dn", l, ex, db))
                    for cc in range(4):
                        d2 = db * 4 + cc
                        for (c0, n) in gout:
                            b = nb()

                            def mmdn(e, s=s, cc=cc, b=b, c0=c0, n=n):
                                for fk in range(8):
                                    o = fk * 512 + cc * 128
                                    ins = e.matmul(ps[b][:, 0:n], lhsT=wsl[s][:, o:o + 128], rhs=actT[:, fk, c0:c0 + n], start=(fk == 0), stop=(fk == 7))
                                return ins
                            S.op("pe", mmdn, reads=[wkey(s)] + [("actT", fk, c0) for fk in range(8)], writes=[("ps", b)])
                            for (s0, sn, kind) in segs_of(c0, n):
                                t = 0 if kind == "x" else 1
                                S.op("dve", lambda e, b=b, d2=d2, c0=c0, s0=s0, sn=sn, mc=modcol(t, 5, d2, l): e.scalar_tensor_tensor(
                                    out=xT[:, d2, s0:s0 + sn], in0=ps[b][:, s0 - c0:s0 - c0 + sn], scalar=mc, in1=xT[:, d2, s0:s0 + sn],
                                    op0=ALU.mult, op1=ALU.add), reads=[("ps", b), ("modp", 0), ("modp", 1)] + xkeys(d2, c0, n), writes=xkeys(d2, c0, n))
            S.barrier()
            if stop_after == "moe%d" % l:
                break

        allx = [("xT", k, g) for k in range(16) for g in range(3)]
        if stop_after is not None:
            S.op("sp", lambda e: e.dma_start(out=dbg_d, in_=A[:, 0:16 * NRES * 4].bitcast(F32)), reads=allx, writes=["dbg"], dma="dbg")
            S.op("sp", None, reads=["dbg"])
        else:
            fgr = [(0, 512), (512, 512)]
            norm_phase(fgr, lambda k, c0, n: xT[:, k, c0:c0 + n], xkeys, 0, 0, final=True)
            for (c0, n) in fgr:
                for k in range(16):
                    S.op("dve", lambda e, k=k, c0=c0, n=n: e.scalar_tensor_tensor(out=xT[:, k, c0:c0 + n], in0=xT[:, k, c0:c0 + n], scalar=V("fg", 0, k),
                                                                                 in1=rstd[:, c0:c0 + n], op0=ALU.mult, op1=ALU.mult),
                         reads=xkeys(k, c0, n) + [("rstd", c0), "vecs"], writes=xkeys(k, c0, n))
            ostg = view(C, 0, F32, [2, D])
            for t in range(8):
                ob = t % 2
                for k4 in range(4):
                    b = nb()

                    def tro(e, t=t, k4=k4, b=b):
                        for kk in range(4):
                            ins = e.transpose(out=ps[b][:, kk * 128:(kk + 1) * 128], in_=xT[:, k4 * 4 + kk, t * 128:(t + 1) * 128], identity=ident[:])
                        return ins
                    S.op("pe", tro, reads=[("xT", k4 * 4 + kk, t // 4) for kk in range(4)] + ["ident"], writes=[("ps", b)])
                    S.op("act" if k4 % 2 == 0 else "dve",
                         (lambda e, ob=ob, k4=k4, b=b: e.activation(out=ostg[:, ob, k4 * 512:(k4 + 1) * 512], in_=ps[b][:], func=AF.Copy))
                         if k4 % 2 == 0 else
                         (lambda e, ob=ob, k4=k4, b=b: e.tensor_copy(out=ostg[:, ob, k4 * 512:(k4 + 1) * 512], in_=ps[b][:])),
                         reads=[("ps", b)], writes=[("ostg", ob, k4)])
                S.op("sp", lambda e, t=t, ob=ob: e.dma_start(out=out_d[t * 128:(t + 1) * 128, :], in_=ostg[:, ob, :]),
                     reads=[("ostg", ob, k4) for k4 in range(4)], writes=[("out", t)], dma="out%d" % ob)
            S.op("sp", None, reads=[("out", t) for t in range(8)])
        S.emit(st)
        nc._sched_stats = (len(S.ops), S.ecount)
        nc._plan = plan
    return nc


def host_inputs(inp, plan, ncores=8):
    x = np.asarray(inp["x"], np.float32)
    ctx = np.asarray(inp["ctx"], np.float32)
    c = np.asarray(inp["c"], np.float32)
    c_ctx = np.asarray(inp["c_ctx"], np.float32)
    wts = pack_weights({k: np.asarray(v) for k, v in inp.items()}, plan)
    half = 32
    inv = (1.0 / (10000.0 ** (np.arange(0, half, 2, dtype=np.float32) / half))).astype(np.float32)
    colf = lambda v: np.ascontiguousarray(np.asarray(v, np.float32).reshape(-1, 128).T)
    maps = []
    for core in range(ncores):
        b, typ = core // 2, core % 2
        pos = np.arange(1280) if typ == 0 else 2047 - np.arange(1280)
        xs = x[b][pos]
        cx = ctx[b] if typ == 0 else ctx[b][::-1]
        xin = np.concatenate([xs[:1152], cx, xs[1152:1280]], axis=0)
        posc = np.concatenate([pos[:1152], np.zeros(256, np.int64), pos[1152:1280]])
        row = (posc // 64).astype(np.float32)
        colp = (posc % 64).astype(np.float32)
        Ct = np.ones((128, NCOL), np.float32)
        St = np.zeros((128, NCOL), np.float32)
        for p in range(128):
            d = p % 64
            f = (d % 32) % 16
            ang = (row if d < 32 else colp) * inv[f]
            Ct[p] = np.cos(ang)
            sn = np.sin(ang)
            St[p] = -sn if (d % 32) < 16 else sn
        Ct[:, 1152:1408] = 1.0
        St[:, 1152:1408] = 0.0
        rope = np.concatenate([Ct, St], axis=1)
        jj = np.arange(128)[:, None]
        ii = np.arange(128)[None, :]
        mp = (ii <= jj).astype(np.float32)
        mn = (jj <= ii).astype(np.float32)
        masks = np.concatenate([np.tile(mp, (1, 4)), np.tile(mn, (1, 4))], axis=1)
        cin = np.stack([colf(c[b]), colf(c_ctx)], axis=2).reshape(128, 32)
        vecs = np.zeros((128, NVEC), np.float32)
        rowv = np.zeros((2, 1536), np.float32)
        sguw = np.zeros((2 * 128, 512), np.float32)
        for l in range(2):
            vecs[:, VOFF[("adab", l)]:VOFF[("adab", l)] + 96] = colf(inp["ada_b"][l])
            vecs[:, VOFF[("n1g", l)]:VOFF[("n1g", l)] + 16] = colf(inp["norm1_g"][l])
            vecs[:, VOFF[("n2g", l)]:VOFF[("n2g", l)] + 16] = colf(inp["norm2_g"][l])
            sk = np.asarray(inp["attn_sink"][l], np.float32)
            for j in range(4):
                vecs[0:64, VOFF[("sink", l)] + j] = sk[2 * j]
                vecs[64:128, VOFF[("sink", l)] + j] = sk[2 * j + 1]
            cdw = np.asarray(inp["conf_dw_w"][l], np.float32)
            scw = np.asarray(inp["sconv_w"][l], np.float32)
            if typ == 1:
                cdw = cdw[::-1]
                scw = scw[::-1]
            for j in range(4):
                vecs[:, VOFF[("cdw", l)] + j * 31:VOFF[("cdw", l)] + (j + 1) * 31] = cdw[:, j * 128:(j + 1) * 128].T
                vecs[:, VOFF[("scw", l)] + j * 3:VOFF[("scw", l)] + (j + 1) * 3] = scw[:, j * 128:(j + 1) * 128].T
            vecs[:, VOFF[("cdb", l)]:VOFF[("cdb", l)] + 4] = colf(inp["conf_dw_b"][l])
            vecs[:, VOFF[("clg", l)]:VOFF[("clg", l)] + 4] = colf(inp["conf_ln_g"][l])
            vecs[:, VOFF[("clb", l)]:VOFF[("clb", l)] + 4] = colf(inp["conf_ln_b"][l])
            rowv[l, 0:512] = inp["sgu_ln_g"][l]
            rowv[l, 512:1024] = inp["sgu_ln_b"][l]
            ws = np.asarray(inp["sgu_w"][l], np.float32)
            bs = np.asarray(inp["sgu_b"][l], np.float32)
            if typ == 1:
                ws = ws[:, ::-1, ::-1]
                bs = bs[:, ::-1]
            rowv[l, 1024:1536] = bs.reshape(-1)
            sguw[l * 128:(l + 1) * 128] = ws.transpose(2, 0, 1).reshape(128, 512)
        vecs[:, VOFF[("fg", 0)]:VOFF[("fg", 0)] + 16] = colf(inp["final_g"])
        rwl = np.asarray(inp["router_w"], np.float32).reshape(16, 128, 16).transpose(1, 0, 2).reshape(128, 256)
        maps.append(dict(xin=np.ascontiguousarray(xin), wts=wts, rope=rope, masks=masks, cin=np.ascontiguousarray(cin),
                         vecs=vecs, rowv=rowv, sguw=sguw, rw=np.ascontiguousarray(rwl),
                         rb=np.asarray(inp["router_b"], np.float32).reshape(1, 16)))
    return maps


def kernel(**inputs):
    nc = build()
    assert len(nc._plan) == NBLK
    maps = host_inputs(inputs, nc._plan)
    res = run_bass_kernel_spmd(nc, maps, core_ids=list(range(8)))
    out = np.zeros((4, 2048, D), np.float32)
    for core in range(8):
        b, typ = core // 2, core % 2
        o = res.results[core]["out"]
        if typ == 0:
            out[b, 0:1024] = o
        else:
            out[b, 1024:2048] = o[::-1]
    return out
```
